# Optimizing a Trainium2 kernel written in Bass

```python
import math
import jax
import jax.numpy as jnp
from jax import lax
import numpy as np

D_MODEL = 1024
BATCH = 32
SEQ = 2048
DEPTH = 2

D_MIX = D_MODEL
D_GROUP = D_MIX // 4

HY_PROJ = 3 * D_GROUP
HY_SHORT = 3
HY_POS_DIM = 33
HY_FILT_HIDDEN = 64
HY_FAST_DECAY_PCT = 0.3
HY_SLOW_DECAY_PCT = 1.5
HY_DECAY_TARGET = 1e-2

M_HEADS = 4
M_HEADDIM = D_GROUP // M_HEADS
M_STATE = 64
M_GROUPS = 2
M_CONV = 5
M_CHUNK = 128
M_XBC = D_GROUP + 2 * M_GROUPS * M_STATE
M_PROJ = D_GROUP + M_XBC + 2 * M_HEADS

A_HEADS = 4
A_HEADDIM = D_GROUP // A_HEADS
A_PATTERNS = ((128, 1), (512, 4), (2048, 16))
A_PROJ = 3 * D_GROUP
N_BUCKETS = 32
MAX_DISTANCE = 1024

H_HEADS = 4
H_EXPAND = D_GROUP // H_HEADS
H_HEADDIM = D_GROUP // H_HEADS
H_CHUNK = 32
H_PROJ = 5 * D_GROUP

N_IN = HY_PROJ + M_PROJ + A_PROJ + H_PROJ

N_EXPERTS = 16
EC_CAPACITY = 2
D_FF_EXPERT = 1024

EPS = 1e-6
F32 = jnp.float32

kernel_name = "hybrid_parallel_heads_encoder"


def rmsnorm(x, w):
    xf = x.astype(F32)
    r = lax.rsqrt(jnp.mean(xf * xf, axis=-1, keepdims=True) + EPS)
    return (xf * r).astype(x.dtype) * w


def centered_dwconv(u, w):
    k = w.shape[0]
    c = u.shape[-1]
    return lax.conv_general_dilated(
        u, w[:, None, :].astype(u.dtype), window_strides=(1,),
        padding=((k // 2, k // 2),), dimension_numbers=("NWC", "WIO", "NWC"),
        feature_group_count=c)


def hyena_filters(L, w1, b1, w2, b2, freq, w3):
    t = jnp.linspace(0.0, 1.0, L, dtype=F32)[:, None]
    bands = (HY_POS_DIM - 1) // 2
    ang_pos = 2.0 * math.pi * jnp.arange(L, dtype=F32) / L
    f = jnp.linspace(1e-4, bands - 1, bands, dtype=F32)
    ang = ang_pos[:, None] * f[None, :]
    z = jnp.concatenate([t, jnp.cos(ang), -jnp.sin(ang)], axis=-1)
    freq = freq.astype(F32)
    h = jnp.sin(freq * (z @ w1.astype(F32) + b1.astype(F32)))
    h = jnp.sin(freq * (h @ w2.astype(F32) + b2.astype(F32)))
    h = h @ w3.astype(F32)
    max_decay = math.log(HY_DECAY_TARGET) / HY_FAST_DECAY_PCT
    min_decay = math.log(HY_DECAY_TARGET) / HY_SLOW_DECAY_PCT
    deltas = jnp.abs(jnp.linspace(min_decay, max_decay, D_GROUP, dtype=F32))
    decay = jnp.exp(-t * deltas[None, :])
    h = h.reshape(L, 2, D_GROUP) * decay[:, None, :]
    return h[:, 0], h[:, 1]


def bidir_fftconv(u, h_fwd, h_bwd, bias):
    L = u.shape[1]
    k = jnp.concatenate([h_fwd, jnp.zeros_like(h_fwd[:1]), h_bwd[:0:-1]], axis=0)
    kf = jnp.fft.rfft(k, n=2 * L, axis=0)
    uf = jnp.fft.rfft(u, n=2 * L, axis=1)
    y = jnp.fft.irfft(uf * kf[None], n=2 * L, axis=1)[:, :L]
    return y + u * bias.astype(F32)


def hyena_mixer(p, conv_w, w1, b1, w2, b2, freq, w3, fbias):
    L = p.shape[1]
    u = centered_dwconv(p, conv_w).astype(F32)
    x0, x1, v = jnp.split(u, 3, axis=-1)
    hf, hb = hyena_filters(L, w1, b1, w2, b2, freq, w3)
    y = x0 * bidir_fftconv(v * x1, hf, hb, fbias)
    return y.astype(p.dtype)


def ssd_chunked(x, dt, A, Bm, Cm):
    b, L, h, pd = x.shape
    n = Bm.shape[-1]
    nc = L // M_CHUNK
    xd = (x * dt[..., None]).reshape(b, nc, M_CHUNK, h, pd)
    Bc = Bm.reshape(b, nc, M_CHUNK, h, n)
    Cc = Cm.reshape(b, nc, M_CHUNK, h, n)
    a_cum = jnp.cumsum((dt * A).reshape(b, nc, M_CHUNK, h), axis=2)
    seg = a_cum[:, :, :, None, :] - a_cum[:, :, None, :, :]
    causal = jnp.tril(jnp.ones((M_CHUNK, M_CHUNK), bool))[None, None, :, :, None]
    Lmat = jnp.exp(jnp.where(causal, seg, -jnp.inf))
    cb = jnp.einsum("bclhn,bcshn->bclsh", Cc, Bc) * Lmat
    y_diag = jnp.einsum("bclsh,bcshp->bclhp", cb, xd)
    decay_to_end = jnp.exp(a_cum[:, :, -1:, :] - a_cum)
    chunk_states = jnp.einsum("bclhn,bclhp->bchpn", Bc, xd * decay_to_end[..., None])
    chunk_decay = jnp.exp(a_cum[:, :, -1, :])

    def step(S, inp):
        st, dec = inp
        return S * dec[..., None, None] + st, S

    S0 = jnp.zeros((b, h, pd, n), F32)
    _, prev = lax.scan(step, S0, (jnp.moveaxis(chunk_states, 1, 0), jnp.moveaxis(chunk_decay, 1, 0)))
    prev = jnp.moveaxis(prev, 0, 1)
    y_off = jnp.einsum("bclhn,bchpn->bclhp", Cc * jnp.exp(a_cum)[..., None], prev)
    return (y_diag + y_off).reshape(b, L, h, pd)


def mamba2_mixer(p, conv_w, conv_b, dt_bias, A_log, Dskip, norm_w):
    b, L, _ = p.shape
    z, xbc, dt_raw = jnp.split(p, [D_GROUP, D_GROUP + M_XBC], axis=-1)
    xbc = jax.nn.silu(centered_dwconv(xbc, conv_w) + conv_b).astype(F32)
    xs, Bm, Cm = jnp.split(xbc, [D_GROUP, D_GROUP + M_GROUPS * M_STATE], axis=-1)
    xs = xs.reshape(b, L, M_HEADS, M_HEADDIM)
    rep = M_HEADS // M_GROUPS
    Bm = jnp.repeat(Bm.reshape(b, L, M_GROUPS, M_STATE), rep, axis=2)
    Cm = jnp.repeat(Cm.reshape(b, L, M_GROUPS, M_STATE), rep, axis=2)
    dt = jax.nn.softplus(dt_raw.astype(F32).reshape(b, L, 2, M_HEADS) + dt_bias.astype(F32))
    A = -jnp.exp(A_log.astype(F32))
    fl = lambda t: jnp.flip(t, axis=1)
    y_f = ssd_chunked(xs, dt[:, :, 0], A[0], Bm, Cm)
    y_b = fl(ssd_chunked(fl(xs), fl(dt[:, :, 1]), A[1], fl(Bm), fl(Cm)))
    y = y_f + y_b + xs * Dskip.astype(F32)[:, None]
    y = y.reshape(b, L, D_GROUP) * jax.nn.silu(z.astype(F32))
    return rmsnorm(y, norm_w.astype(F32)).astype(p.dtype)


def t5_bucket(rel):
    nb = N_BUCKETS // 2
    max_exact = nb // 2
    ret = (rel > 0).astype(jnp.int32) * nb
    n = jnp.abs(rel)
    nf = jnp.maximum(n, 1).astype(F32)
    large = max_exact + (jnp.log(nf / max_exact) / math.log(MAX_DISTANCE / max_exact)
                         * (nb - max_exact)).astype(jnp.int32)
    large = jnp.minimum(large, nb - 1)
    return ret + jnp.where(n < max_exact, n, large)


def band_attention_parts(q, k, v, rel_bias, dil, band):
    b, h, L, dh = q.shape
    n = L // dil
    nb = -(-n // band)
    npad = nb * band

    def to_sub(t):
        t = t.reshape(b, h, n, dil, dh).transpose(0, 1, 3, 2, 4)
        return jnp.pad(t, ((0, 0), (0, 0), (0, 0), (0, npad - n), (0, 0)))

    def kv_band(t):
        tp = jnp.pad(to_sub(t), ((0, 0), (0, 0), (0, 0), (band, band), (0, 0)))
        tp = tp.reshape(b, h, dil, nb + 2, band, dh)
        return jnp.concatenate([tp[:, :, :, :-2], tp[:, :, :, 1:-1], tp[:, :, :, 2:]], axis=4)

    qs = to_sub(q).reshape(b, h, dil, nb, band, dh)
    kb = kv_band(k)
    vb = kv_band(v)
    rel = jnp.arange(3 * band)[None, :] - band - jnp.arange(band)[:, None]
    bias = jnp.transpose(rel_bias.astype(F32)[t5_bucket(rel * dil)], (2, 0, 1))
    kidx = jnp.arange(nb)[:, None, None] * band + band + rel[None] - band + jnp.arange(band)[None, :, None]
    valid = (jnp.abs(rel)[None] <= band) & (kidx >= 0) & (kidx < n)
    logits = jnp.einsum("bhrnqd,bhrnkd->bhrnqk", qs, kb) + bias[None, :, None, None]
    logits = jnp.where(valid, logits, -jnp.inf)
    m = jnp.max(logits, axis=-1)
    m = jnp.where(jnp.isfinite(m), m, 0.0)
    pr = jnp.exp(logits - m[..., None])
    den = jnp.sum(pr, axis=-1)
    num = jnp.einsum("bhrnqk,bhrnkd->bhrnqd", pr, vb)

    def from_sub(t):
        t = t.reshape(b, h, dil, npad, *t.shape[5:])[:, :, :, :n]
        return jnp.moveaxis(t, 2, 3).reshape(b, h, L, *t.shape[4:])

    return from_sub(num), from_sub(den), from_sub(m)


def dilated_attention_mixer(p, rel_bias):
    b, L, _ = p.shape
    q, k, v = jnp.split(p.astype(F32), 3, axis=-1)
    heads = lambda t: t.reshape(b, L, A_HEADS, A_HEADDIM).transpose(0, 2, 1, 3)
    q, k, v = heads(q) * (A_HEADDIM ** -0.5), heads(k), heads(v)
    parts = [band_attention_parts(q, k, v, rel_bias, d, w // (2 * d)) for (w, d) in A_PATTERNS]
    m_all = jnp.max(jnp.stack([pt[2] for pt in parts]), axis=0)
    num = 0.0
    den = 0.0
    for n_i, d_i, m_i in parts:
        w_i = jnp.exp(m_i - m_all)
        num = num + w_i[..., None] * n_i
        den = den + w_i * d_i
    o = num / den[..., None]
    return o.transpose(0, 2, 1, 3).reshape(b, L, D_GROUP).astype(p.dtype)


def gla_chunked(q, k, v, g):
    b, L, h, e = q.shape
    pd = v.shape[-1]
    nc = L // H_CHUNK
    rs = lambda t: t.reshape(b, nc, H_CHUNK, h, t.shape[-1])
    q, k, v, g = rs(q), rs(k), rs(v), rs(g)
    gc = jnp.cumsum(g, axis=2)
    ref = gc[:, :, H_CHUNK // 2 - 1:H_CHUNK // 2]
    scores = jnp.einsum("bclhe,bcshe->bchls", q * jnp.exp(gc - ref), k * jnp.exp(ref - gc))
    mask = jnp.tril(jnp.ones((H_CHUNK, H_CHUNK), bool))
    scores = jnp.where(mask, scores, 0.0)
    o_intra = jnp.einsum("bchls,bcshp->bclhp", scores, v)
    g_last = gc[:, :, -1:]
    U = jnp.einsum("bclhe,bclhp->bchep", k * jnp.exp(g_last - gc), v)
    a = jnp.exp(g_last[:, :, 0])

    def step(S, inp):
        u_c, a_c = inp
        return a_c[..., None] * S + u_c, S

    S0 = jnp.zeros((b, h, e, pd), F32)
    _, prev = lax.scan(step, S0, (jnp.moveaxis(U, 1, 0), jnp.moveaxis(a, 1, 0)))
    prev = jnp.moveaxis(prev, 0, 1)
    o_inter = jnp.einsum("bclhe,bchep->bclhp", q * jnp.exp(gc), prev)
    return (o_intra + o_inter).reshape(b, L, h, pd)


def hgrn2_mixer(p, lb, norm_w):
    b, L, _ = p.shape
    q, f_fwd, f_bwd, i, g = jnp.split(p.astype(F32), 5, axis=-1)
    hs = lambda t, d: t.reshape(b, L, H_HEADS, d)
    q = hs(jax.nn.silu(q), H_EXPAND)
    i = hs(i, H_HEADDIM)
    lb = lb.reshape(2, H_HEADS, H_EXPAND)

    def gates(fpre, lbd):
        fpre = hs(fpre, H_EXPAND)
        f = lbd + (1.0 - lbd) * jax.nn.sigmoid(fpre)
        return jnp.log(f), (1.0 - lbd) * jax.nn.sigmoid(-fpre)

    gf, kf = gates(f_fwd, lb[0])
    gb, kb = gates(f_bwd, lb[1])
    fl = lambda t: jnp.flip(t, axis=1)
    o = gla_chunked(q, kf, i, gf) + fl(gla_chunked(fl(q), fl(kb), fl(i), fl(gb)))
    o = rmsnorm(o, norm_w.astype(F32)) * jax.nn.silu(hs(g, H_HEADDIM))
    return o.reshape(b, L, D_GROUP).astype(p.dtype)


def expert_choice_moe(x, w_router, w_gate, w_up, w_down):
    b, L, d = x.shape
    cap = EC_CAPACITY * L // N_EXPERTS
    aff = jax.nn.softmax((x @ w_router).astype(F32), axis=-1)
    gate, idx = lax.top_k(jnp.swapaxes(aff, 1, 2), cap)
    xe = jax.vmap(lambda xb, ib: xb[ib])(x, idx)
    hdn = jax.nn.silu(jnp.einsum("becd,edf->becf", xe, w_gate)) * jnp.einsum("becd,edf->becf", xe, w_up)
    ye = jnp.einsum("becf,efd->becd", hdn, w_down) * gate[..., None].astype(x.dtype)
    return jax.vmap(lambda ib, yb: jnp.zeros((L, d), yb.dtype).at[ib.reshape(-1)].add(yb.reshape(-1, d)))(idx, ye)


def setup_inputs(seed: int = 0) -> dict:
    key = jax.random.key(seed)
    ks = jax.random.split(key, 27)
    nrm = lambda k, shape, std: std * jax.random.normal(k, shape, F32)
    gain = lambda k, shape: 1.0 + 0.05 * jax.random.normal(k, shape, F32)
    dt0 = jnp.exp(jax.random.uniform(ks[15], (DEPTH, 2, M_HEADS), F32, math.log(1e-3), math.log(1e-1)))
    return {
        "x": jax.random.normal(ks[0], (BATCH, SEQ, D_MODEL), F32),
        "w_in": nrm(ks[1], (DEPTH, D_MODEL, N_IN), D_MODEL ** -0.5),
        "w_out": nrm(ks[2], (DEPTH, D_MIX, D_MODEL), D_MIX ** -0.5),
        "norm_mix_w": gain(ks[3], (DEPTH, D_MODEL)),
        "norm_ffn_w": gain(ks[4], (DEPTH, D_MODEL)),
        "hy_conv_w": nrm(ks[5], (DEPTH, HY_SHORT, HY_PROJ), HY_SHORT ** -0.5),
        "hy_pos_w1": nrm(ks[6], (DEPTH, HY_POS_DIM, HY_FILT_HIDDEN), HY_POS_DIM ** -0.5),
        "hy_pos_b1": nrm(ks[7], (DEPTH, HY_FILT_HIDDEN), 0.1),
        "hy_pos_w2": nrm(ks[8], (DEPTH, HY_FILT_HIDDEN, HY_FILT_HIDDEN), HY_FILT_HIDDEN ** -0.5),
        "hy_pos_b2": nrm(ks[9], (DEPTH, HY_FILT_HIDDEN), 0.1),
        "hy_sin_freq": gain(ks[10], (DEPTH, HY_FILT_HIDDEN)),
        "hy_pos_w3": nrm(ks[11], (DEPTH, HY_FILT_HIDDEN, 2 * D_GROUP), 0.1 * HY_FILT_HIDDEN ** -0.5),
        "hy_filt_bias": nrm(ks[12], (DEPTH, D_GROUP), 1.0),
        "m_conv_w": nrm(ks[13], (DEPTH, M_CONV, M_XBC), M_CONV ** -0.5),
        "m_conv_b": nrm(ks[14], (DEPTH, M_XBC), 0.02),
        "m_dt_bias": dt0 + jnp.log(-jnp.expm1(-dt0)),
        "m_A_log": jnp.log(jax.random.uniform(ks[16], (DEPTH, 2, M_HEADS), F32, 1.0, 16.0)),
        "m_D": gain(ks[17], (DEPTH, M_HEADS)),
        "m_norm_w": gain(ks[18], (DEPTH, D_GROUP)),
        "rel_bias": nrm(ks[19], (N_BUCKETS, A_HEADS), 0.5),
        "hg_lb": nrm(ks[20], (DEPTH, 2, D_GROUP), 1.0),
        "hg_norm_w": gain(ks[21], (DEPTH, H_HEADDIM)),
        "router_w": nrm(ks[22], (DEPTH, D_MODEL, N_EXPERTS), D_MODEL ** -0.5),
        "moe_w_gate": nrm(ks[23], (DEPTH, N_EXPERTS, D_MODEL, D_FF_EXPERT), D_MODEL ** -0.5),
        "moe_w_up": nrm(ks[24], (DEPTH, N_EXPERTS, D_MODEL, D_FF_EXPERT), D_MODEL ** -0.5),
        "moe_w_down": nrm(ks[25], (DEPTH, N_EXPERTS, D_FF_EXPERT, D_MODEL), D_FF_EXPERT ** -0.5),
        "final_norm_w": gain(ks[26], (D_MODEL,)),
    }


def reference(x, w_in, w_out, norm_mix_w, norm_ffn_w, hy_conv_w, hy_pos_w1, hy_pos_b1, hy_pos_w2,
              hy_pos_b2, hy_sin_freq, hy_pos_w3, hy_filt_bias, m_conv_w, m_conv_b, m_dt_bias, m_A_log,
              m_D, m_norm_w, rel_bias, hg_lb, hg_norm_w, router_w, moe_w_gate, moe_w_up, moe_w_down,
              final_norm_w):
    sm = jax.nn.softmax(hg_lb.astype(F32), axis=0)
    lower_bounds = jnp.cumsum(sm, axis=0) - sm[:1]
    splits = [HY_PROJ, HY_PROJ + M_PROJ, HY_PROJ + M_PROJ + A_PROJ]
    for l in range(DEPTH):
        hn = rmsnorm(x, norm_mix_w[l])
        proj = hn @ w_in[l]
        pa, pb, pc, pd = jnp.split(proj, splits, axis=-1)
        ya = hyena_mixer(pa, hy_conv_w[l], hy_pos_w1[l], hy_pos_b1[l], hy_pos_w2[l], hy_pos_b2[l],
                         hy_sin_freq[l], hy_pos_w3[l], hy_filt_bias[l])
        yb = mamba2_mixer(pb, m_conv_w[l], m_conv_b[l], m_dt_bias[l], m_A_log[l], m_D[l], m_norm_w[l])
        yc = dilated_attention_mixer(pc, rel_bias)
        yd = hgrn2_mixer(pd, lower_bounds[l], hg_norm_w[l])
        mix = jnp.concatenate([ya, yb, yc, yd], axis=-1).astype(x.dtype)
        x = x + mix @ w_out[l]
        x = x + expert_choice_moe(rmsnorm(x, norm_ffn_w[l]), router_w[l], moe_w_gate[l],
                                  moe_w_up[l], moe_w_down[l])
    return rmsnorm(x, final_norm_w)
```

```python
import numpy as np
import ml_dtypes
from contextlib import ExitStack
import concourse.bass as bass
import concourse.mybir as mybir
from concourse.bass_utils import run_bass_kernel_spmd

F32 = mybir.dt.float32
BF16 = mybir.dt.bfloat16
I32 = mybir.dt.int32
U32 = mybir.dt.uint32
AF = mybir.ActivationFunctionType
ALU = mybir.AluOpType
AX = mybir.AxisListType


class Ref:
    __slots__ = ("ap", "tile", "key")

    def __init__(self, ap, tile, key):
        self.ap, self.tile, self.key = ap, tile, key

    def __getitem__(self, idx):
        return Ref(self.ap[idx], self.tile, self.key)

    def rr(self, pat, **kw):
        return Ref(self.ap.rearrange(pat, **kw), self.tile, self.key)

    def bc(self, shape):
        return Ref(self.ap.to_broadcast(list(shape)), self.tile, self.key)

    def bcast(self, shape):
        return Ref(self.ap.broadcast_to(list(shape)), self.tile, self.key)

    def pbc(self, n):
        return Ref(self.ap.partition_broadcast(n), self.tile, self.key)

    def bitcast(self, dt):
        return Ref(self.ap.bitcast(dt), self.tile, self.key)

    def unsq(self, ax):
        return Ref(self.ap.unsqueeze(ax), self.tile, self.key)

    def with_key(self, key):
        return Ref(self.ap, self.tile, key)


class _Keyed:
    def __init__(self, tile, key):
        self.tile, self.key = tile, key

    def __getitem__(self, idx):
        return Ref(self.tile._ap()[idx], self.tile, self.key)


class Tile:
    def __init__(self, handle, name, space):
        self.h, self.name, self.space = handle, name, space
        self.state = {}

    def _ap(self):
        return self.h.ap() if self.space == "dram" else self.h

    def __getitem__(self, idx):
        return Ref(self._ap()[idx], self, None)

    def k(self, key):
        return _Keyed(self, key)

    @property
    def all(self):
        return self[:]


class Instr:
    __slots__ = ("i", "eng", "fn", "deps", "dma", "signal", "sigidx", "dsem", "dval")

    def __init__(self, i, eng, fn, deps, dma):
        self.i, self.eng, self.fn, self.deps, self.dma = i, eng, fn, deps, dma
        self.signal = False
        self.sigidx = 0
        self.dsem = None
        self.dval = 0


ENGS = ("pe", "act", "dve", "pool", "sp")
DMAQ = ("sp", "act", "pool")
NDSEM = 8


class Prog:
    def __init__(self):
        self.nc = bass.Bass("TRN2", target_bir_lowering=False)
        self.instrs = []
        self.stack = ExitStack()
        self.pstack = None
        self.phase_start = 0
        nc = self.nc
        st = self.stack
        self.engsem = {e: st.enter_context(nc.semaphore("sem_" + e)) for e in ENGS}
        self.dsems = {e: [st.enter_context(nc.semaphore("dsem_%s_%d" % (e, j))) for j in range(NDSEM)]
                      for e in DMAQ}
        self.sigcount = {e: 0 for e in ENGS}
        self.dcount = {e: 0 for e in DMAQ}
        self.seen = {e: {} for e in ENGS}
        self._uid = 0

    def dram(self, name, shape, dtype, kind="Internal"):
        h = self.nc.dram_tensor(name, list(shape), dtype, kind=kind)
        return Tile(h, name, "dram")

    def _st(self):
        return self.pstack if self.pstack is not None else self.stack

    def sb(self, name, shape, dtype):
        self._uid += 1
        nm = "%s_%d" % (name, self._uid)
        h = self._st().enter_context(self.nc.sbuf_tensor(nm, list(shape), dtype))
        return Tile(h, nm, "sb")

    def ps(self, name, shape, dtype=F32):
        self._uid += 1
        nm = "%s_%d" % (name, self._uid)
        h = self._st().enter_context(self.nc.psum_tensor(nm, list(shape), dtype))
        return Tile(h, nm, "ps")

    def begin(self, name):
        assert self.pstack is None
        self.pstack = ExitStack()
        self.phase_start = len(self.instrs)
        self.phase_name = name

    def end(self):
        self._emit_phase()
        self.pstack.close()
        self.pstack = None

    @staticmethod
    def _overlap(state, key):
        if key is None:
            return list(state.keys())
        return [k for k in state.keys() if k is None or k == key]

    def _rec(self, eng, fn, reads, writes, dma=False):
        i = len(self.instrs)
        ps0 = self.phase_start
        deps = set()
        for r in reads:
            st = r.tile.state
            for k in self._overlap(st, r.key):
                w = st[k][0]
                if w is not None and w >= ps0:
                    deps.add(w)
        for w_ in writes:
            st = w_.tile.state
            for k in self._overlap(st, w_.key):
                e = st[k]
                if e[0] is not None and e[0] >= ps0:
                    deps.add(e[0])
                for rr_ in e[1]:
                    if rr_ >= ps0:
                        deps.add(rr_)
        for r in reads:
            st = r.tile.state
            e = st.get(r.key)
            if e is None:
                e = st[r.key] = [None, []]
            e[1] = [q for q in e[1] if q >= ps0]
            e[1].append(i)
        for w_ in writes:
            st = w_.tile.state
            if w_.key is None:
                st.clear()
                st[None] = [i, []]
            else:
                st[w_.key] = [i, []]
        deps.discard(i)
        ins = Instr(i, eng, fn, deps, dma)
        self.instrs.append(ins)
        return ins

    @staticmethod
    def _u(x):
        return x.ap if isinstance(x, Ref) else x

    @staticmethod
    def _refs(*xs):
        return [x for x in xs if isinstance(x, Ref)]

    def mm(self, out, lhsT, rhs, start=True, stop=True):
        u = self._u
        return self._rec("pe", lambda e: e.matmul(u(out), u(lhsT), u(rhs), start=start, stop=stop),
                         self._refs(lhsT, rhs), self._refs(out))

    def transpose(self, out, in_, ident):
        u = self._u
        return self._rec("pe", lambda e: e.transpose(u(out), u(in_), u(ident)),
                         self._refs(in_, ident), self._refs(out))

    def act(self, out, in_, func, bias=None, scale=1.0, accum=None, eng="act"):
        u = self._u
        kw = {}
        if bias is not None:
            kw["bias"] = u(bias)
        if accum is not None:
            kw["accum_out"] = u(accum)
        return self._rec(eng, lambda e: e.activation(u(out), u(in_), func, scale=u(scale), **kw),
                         self._refs(in_, bias, scale), self._refs(out, accum))

    def tt(self, eng, out, a, b, op):
        u = self._u
        return self._rec(eng, lambda e: e.tensor_tensor(u(out), u(a), u(b), op),
                         self._refs(a, b), self._refs(out))

    def ts(self, eng, out, a, s1, op0, s2=None, op1=None, accum=None):
        u = self._u
        kw = {}
        if accum is not None:
            kw["accum_out"] = u(accum)
        if op1 is None:
            return self._rec(eng, lambda e: e.tensor_scalar(u(out), u(a), u(s1), None, op0, **kw),
                             self._refs(a, s1), self._refs(out, accum))
        return self._rec(eng, lambda e: e.tensor_scalar(u(out), u(a), u(s1), u(s2), op0, op1, **kw),
                         self._refs(a, s1, s2), self._refs(out, accum))

    def stt(self, out, a, s, b, op0, op1, eng="dve"):
        u = self._u
        return self._rec(eng, lambda e: e.scalar_tensor_tensor(u(out), u(a), u(s), u(b), op0, op1),
                         self._refs(a, s, b), self._refs(out))

    def copy(self, eng, out, in_):
        u = self._u
        if eng == "act":
            return self._rec(eng, lambda e: e.copy(u(out), u(in_)), self._refs(in_), self._refs(out))
        return self._rec(eng, lambda e: e.tensor_copy(u(out), u(in_)), self._refs(in_), self._refs(out))

    def memset(self, eng, out, val):
        u = self._u
        return self._rec(eng, lambda e: e.memset(u(out), val), [], self._refs(out))

    def reduce(self, out, in_, op, axis=AX.X, eng="dve"):
        u = self._u
        return self._rec(eng, lambda e: e.tensor_reduce(u(out), u(in_), axis, op),
                         self._refs(in_), self._refs(out))

    def recip(self, out, in_):
        u = self._u
        return self._rec("dve", lambda e: e.reciprocal(u(out), u(in_)), self._refs(in_), self._refs(out))

    def scan(self, out, d0, d1, init, op0, op1):
        u = self._u
        return self._rec("dve", lambda e: e.tensor_tensor_scan(u(out), u(d0), u(d1), u(init), op0, op1),
                         self._refs(d0, d1, init), self._refs(out))

    def generic(self, eng, fn, reads, writes):
        return self._rec(eng, fn, reads, writes)

    def dma(self, q, out, in_, **kw):
        u = self._u
        return self._rec(q, lambda e: e.dma_start(out=u(out), in_=u(in_), **kw),
                         self._refs(in_), self._refs(out), dma=True)

    def dma_generic(self, q, fn, reads, writes):
        return self._rec(q, fn, reads, writes, dma=True)


    def _emit_phase(self):
        nc = self.nc
        instrs = self.instrs
        ps0 = self.phase_start
        cur = instrs[ps0:]
        per = {e: [] for e in ENGS}
        for ins in cur:
            per[ins.eng].append(ins)
        for ins in cur:
            for d in ins.deps:
                D = instrs[d]
                if D.dma:
                    continue
                if D.eng == "pe" and ins.eng == "pe":
                    continue
                D.signal = True
        for e in ENGS:
            for ins in per[e]:
                if ins.signal:
                    self.sigcount[e] += 1
                    ins.sigidx = self.sigcount[e]
        for e in DMAQ:
            for ins in per[e]:
                if ins.dma:
                    k = self.dcount[e]
                    ins.dsem = self.dsems[e][k % NDSEM]
                    ins.dval = 16 * (k // NDSEM + 1)
                    self.dcount[e] = k + 1
        engsem = self.engsem
        with nc.Block() as block:
            def run(ename, eng):
                seen = self.seen[ename]

                def wait(sem, val):
                    if seen.get(sem.name, 0) >= val:
                        return
                    eng.wait_ge(sem, val)
                    seen[sem.name] = val

                last = {}
                for ins in per[ename]:
                    for d in sorted(ins.deps):
                        D = instrs[d]
                        if D.dma:
                            wait(D.dsem, D.dval)
                        else:
                            if D.eng == "pe" and ename == "pe":
                                continue
                            wait(engsem[D.eng], D.sigidx)
                    if ins.dma and ins.dval > 16:
                        wait(ins.dsem, ins.dval - 16)
                    bi = ins.fn(eng)
                    if ins.dma:
                        bi.then_inc(ins.dsem, 16)
                        last[ins.dsem.name] = (ins.dsem, ins.dval)
                    elif ins.signal:
                        bi.then_inc(engsem[ename], 1)
                for (sem, val) in last.values():
                    wait(sem, val)

            block.tensor(lambda eng: run("pe", eng))
            block.scalar(lambda eng: run("act", eng))
            block.vector(lambda eng: run("dve", eng))
            block.gpsimd(lambda eng: run("pool", eng))
            block.sync(lambda eng: run("sp", eng))

    def finish(self):
        self.stack.close()
        return self.nc

import math

D = 1024
L = 2048
NT = 16
EPS = 1e-6
FM_SEGS = [(0, 768, 0), (1024, 512, 768), (1544, 512, 1280), (2312, 768, 1792), (1536, 8, 2560)]
NFM = 2568
TM_SEGS = [(768, 256, 0), (2056, 256, 256), (3080, 512, 512)]
NTM = 1024
R_HY, R_MX, R_AT, R_HG, R_DT = 0, 768, 1280, 1792, 2560
C_MZ, C_AV, C_HI, C_HGATE = 0, 256, 512, 768


class Rot:
    def __init__(self, tiles):
        self.t, self.i = tiles, 0

    def next(self):
        t = self.t[self.i % len(self.t)]
        self.i += 1
        return t


def make_ident(P, dtype=BF16):
    idf = P.sb("identf", [128, 128], F32)
    P.memset("pool", idf[:], 0.0)
    P.generic("pool", lambda e: e.affine_select(idf.h[:], idf.h[:], [[-1, 128]], ALU.not_equal, 1.0,
                                                base=0, channel_multiplier=1), [idf[:]], [idf[:]])
    if dtype == F32:
        return idf
    idb = P.sb("identb", [128, 128], BF16)
    P.copy("dve", idb[:], idf[:])
    return idb


def rms_rstd(P, ss, rstd, n):
    P.ts("dve", rstd[:], ss[:], 1.0 / n, ALU.mult, EPS, ALU.add)
    P.act(rstd[:], rstd[:], AF.Sqrt)
    P.recip(rstd[:], rstd[:])


def phase_P(P, T, Xin, w_in, nw, pT, ptm):
    P.begin("P")
    Wfm = P.sb("Wfm", [128, 8, NFM], BF16)
    Wtm = P.sb("Wtm", [128, 8, NTM], BF16)
    nwt = P.sb("nwt", [128, 8], F32)
    P.dma("sp", nwt[:], nw[:])
    stg = Rot([P.sb("stg", [128, 3592], F32) for _ in range(3)])
    for k in range(8):
        st = stg.next()
        P.dma("sp" if k % 2 == 0 else "act", st[:], w_in[k * 128:(k + 1) * 128, :])
        segs = [(Wfm, a) for a in FM_SEGS] + [(Wtm, a) for a in TM_SEGS]
        for si, (Wt, (s0, n, d0)) in enumerate(segs):
            eng = ("dve", "act", "pool")[(si + k) % 3]
            if eng == "act":
                P.act(Wt[:, k, d0:d0 + n], st[:, s0:s0 + n], AF.Copy, scale=nwt[:, k:k + 1])
            else:
                P.ts(eng, Wt[:, k, d0:d0 + n], st[:, s0:s0 + n], nwt[:, k:k + 1], ALU.mult)
    ident = make_ident(P)
    xb = Rot([P.sb("xb", [128, D], F32) for _ in range(3)])
    sq = P.sb("sq", [128, D], F32)
    ssb = Rot([P.sb("ss", [128, 1], F32) for _ in range(4)])
    rsb = Rot([P.sb("rs", [128, 1], F32) for _ in range(4)])
    hTb = Rot([P.sb("hT", [128, 8, 512], BF16) for _ in range(2)])
    psT = Rot([P.ps("psT", [128, 8, 128], BF16) for _ in range(2)])
    pstm = Rot([P.ps("pstm", [128, 512], F32) for _ in range(2)])
    psfm = Rot([P.ps("psfm", [128, 512], F32) for _ in range(3)])
    otm = Rot([P.sb("otm", [128, NTM], F32) for _ in range(2)])
    ofm = Rot([P.sb("ofm", [128, 512], F32) for _ in range(3)])
    nch = (NFM + 127) // 128
    hnb = Rot([P.sb("hn8", [128, D], BF16) for _ in range(8)])

    def norms(g):
        res = []
        for i4 in range(4):
            t0 = (g * 4 + i4) * 128
            xt = xb.next()
            P.dma("sp", xt[:], Xin[t0:t0 + 128, :])
            ss, rs, hn = ssb.next(), rsb.next(), hnb.next()
            P.act(sq[:], xt[:], AF.Square, accum=ss[:])
            rms_rstd(P, ss, rs, D)
            P.ts("dve", hn[:], xt[:], rs[:], ALU.mult)
            res.append(hn)
        return res

    ng = T // 512
    hns = norms(0)
    for g in range(ng):
        hT = hTb.next()
        cur_hn = hns
        for i4 in range(4):
            t0 = (g * 4 + i4) * 128
            hn = cur_hn[i4]
            pt = psT.next()
            for k in range(8):
                P.transpose(pt[:, k, :], hn[:, k * 128:(k + 1) * 128], ident[:])
            P.copy("act", hT[:, :, i4 * 128:(i4 + 1) * 128], pt[:])
        if g + 1 < ng:
            hns = norms(g + 1)
        for i4 in range(4):
            t0 = (g * 4 + i4) * 128
            o = otm.next()
            for half in range(2):
                pm = pstm.next()
                for k in range(8):
                    P.mm(pm[:], hT[:, k, i4 * 128:(i4 + 1) * 128], Wtm[:, k, half * 512:(half + 1) * 512],
                         start=(k == 0), stop=(k == 7))
                P.copy("dve" if half == 0 else "act", o[:, half * 512:(half + 1) * 512], pm[:])
            P.dma("pool", ptm[t0:t0 + 128, :], o[:])
        for c in range(nch):
            n = min(128, NFM - c * 128)
            pf = psfm.next()
            for k in range(8):
                P.mm(pf[0:n, :], Wfm[:, k, c * 128:c * 128 + n], hT[:, k, :], start=(k == 0), stop=(k == 7))
            of = ofm.next()
            P.copy("dve" if c % 2 == 0 else "act", of[0:n, :], pf[0:n, :])
            P.dma("pool", pT[c * 128:c * 128 + n, g * 512:(g + 1) * 512], of[0:n, :])
    P.end()


def gla_masks():
    s = np.arange(128)[:, None]
    t = np.arange(128)[None, :]
    same32 = (s // 32) == (t // 32)
    mCf = same32 & (s <= t)
    mCb = same32 & (s >= t)
    mABf = ((s < 64) & (t >= 64)) | ((s < 32) & (t >= 32) & (t < 64)) | ((s >= 64) & (s < 96) & (t >= 96))
    mABb = mABf.T
    return np.stack([np.concatenate([mCf, mABf], 1), np.concatenate([mCb, mABb], 1)]).astype(np.uint32)


def bank(P, name):
    return P.ps(name, [128, 512], F32)


def cpred(P, out, mask, data):
    return P.generic("dve", lambda e: e.copy_predicated(out.ap, mask.ap, data.ap), [mask, data], [out])


def phase_GLA(P, nseq, Ls, gq, gk, gg, gv, go, masks_d):
    P.begin("GLA")
    nt = Ls // 128
    ident = make_ident(P)
    msk = P.sb("msk", [128, 2, 256], U32)
    P.dma("sp", msk[:], masks_d[:].rr("m p t -> p m t"))
    qb = Rot([P.sb("q", [128, Ls], BF16) for _ in range(1)])
    kb = Rot([P.sb("k", [128, Ls], BF16) for _ in range(1)])
    gb = Rot([P.sb("g", [128, Ls], F32) for _ in range(1)])
    Fb = Rot([P.sb("F", [128, Ls + 1], F32) for _ in range(1)])
    vb = Rot([P.sb("v", [128, nt, 128], BF16) for _ in range(2)])
    Dt = Rot([P.sb("Dt", [128, Ls], F32) for _ in range(3)])
    Et = Rot([P.sb("Et", [128, Ls], BF16) for _ in range(4)])
    names = ["qT", "kT", "qA", "kA", "qB", "kB", "qC", "kC"]
    der = {n: Rot([P.sb(n, [128, Ls], BF16) for _ in range(2)]) for n in names}
    ktm = Rot([P.sb("ktm", [128, nt, 128], BF16) for _ in range(2)])
    aT = Rot([P.sb("aT", [128, nt], F32) for _ in range(2)])
    oacc = P.sb("oacc", [128, nt, 256], F32)
    Gt = {d: Rot([P.sb("Gt%d" % d, [128, 256], BF16) for _ in range(4)]) for d in range(2)}
    for d in range(2):
        for t_ in Gt[d].t:
            P.memset("pool", t_[:], 0.0)
    Sin32b = Rot([P.sb("Sin32", [128, nt, 128], F32) for _ in range(2)])
    Sinbfb = Rot([P.sb("Sinbf", [128, nt, 128], BF16) for _ in range(2)])
    psC = Rot([bank(P, "psC") for _ in range(4)])
    psO = Rot([bank(P, "psO") for _ in range(2)])
    psU = bank(P, "psU")
    psK = P.ps("psK", [128, 8, 128], BF16)
    z0 = Gt[0].t[0]
    for pc_ in psC.t:
        P.mm(pc_[:, 0:256], z0[:, 0:128], z0[:, 0:256])
    mul_eng = {"qC": "dve", "kC": "pool", "qB": "dve", "kB": "pool", "qA": "dve", "kA": "pool", "qT": "dve", "kT": "dve"}

    def make_prep(s, d, c):
        c0 = s * Ls
        q, k, g, F, v = qb.next(), kb.next(), gb.next(), Fb.next(), vb.next()
        a, km = aT.next(), ktm.next()
        Sin32, Sinbf = Sin32b.next(), Sinbfb.next()
        dv = {}
        ctx = dict(v=v, a=a, km=km, dv=dv, Sinbf=Sinbf)
        th = []
        rows = slice(c * 128, (c + 1) * 128)
        th.append(lambda: P.dma("sp", q[:], gq[rows, c0:c0 + Ls]))
        th.append(lambda: P.dma("sp", k[:], gk[d, rows, c0:c0 + Ls]))
        th.append(lambda: P.dma("sp", g[:], gg[d, rows, c0:c0 + Ls]))
        th.append(lambda: P.dma("sp", v[:], gv[c0:c0 + Ls, rows].rr("(n p) c -> p n c", p=128)))
        th.append(lambda: P.memset("pool", F[:, 0:1], 0.0))
        th.append(lambda: P.scan(F[:, 1:Ls + 1], g[:], g[:], 0.0, ALU.add, ALU.bypass))
        Fsrc = F[:, 1:Ls + 1] if d == 0 else F[:, 0:Ls]
        sq, sk = (1.0, -1.0) if d == 0 else (-1.0, 1.0)

        def refs(blk, refoff):
            nb = Ls // blk
            return F[:, refoff:refoff + (nb - 1) * blk + 1:blk].unsq(2).bcast([128, nb, blk])

        order = list(range(nt)) if d == 0 else list(range(nt - 1, -1, -1))

        def mk_sub(blk, refoff):
            Dm = Dt.next()
            return Dm, (lambda: P.tt("pool", Dm[:].rr("p (n b) -> p n b", b=blk), Fsrc.rr("p (n b) -> p n b", b=blk),
                                     refs(blk, refoff), ALU.subtract))

        def mk_exp_mul(nm, base, Dm, sgn):
            E = Et.next()
            o = der[nm].next()
            dv[nm] = o
            H = Ls // 2

            def ex():
                P.act(E[:, 0:H], Dm[:, 0:H], AF.Exp, scale=sgn)
                P.act(E[:, H:Ls], Dm[:, H:Ls], AF.Exp, scale=sgn)

            def mu():
                P.tt(mul_eng[nm], o[:, 0:H], base[:, 0:H], E[:, 0:H], ALU.mult)
                P.tt(mul_eng[nm], o[:, H:Ls], base[:, H:Ls], E[:, H:Ls], ALU.mult)
            return ex, mu

        D1, S1 = mk_sub(128, 0 if d == 0 else 128)
        D2, S2 = mk_sub(128, 128 if d == 0 else 0)
        D3 = Dt.next()
        S3 = lambda: P.tt("pool", D3[:, 0:nt], F[:, 128:Ls + 1:128], F[:, 0:Ls:128], ALU.subtract)
        Ea = lambda: P.act(a[:], D3[:, 0:nt], AF.Exp)
        E1, M1 = mk_exp_mul("qT", q, D1, sq)
        E2, M2 = mk_exp_mul("kT", k, D2, sk)
        D4, S4 = mk_sub(32, 16)
        E4q, M4q = mk_exp_mul("qC", q, D4, sq)
        E4k, M4k = mk_exp_mul("kC", k, D4, sk)
        D5, S5 = mk_sub(64, 32)
        E5q, M5q = mk_exp_mul("qB", q, D5, sq)
        E5k, M5k = mk_exp_mul("kB", k, D5, sk)
        D6, S6 = mk_sub(128, 64)
        E6q, M6q = mk_exp_mul("qA", q, D6, sq)
        E6k, M6k = mk_exp_mul("kA", k, D6, sk)
        tr = []
        for T4 in range(0, nt, 8):
            n8 = min(8, nt - T4)
            for j in range(n8):
                tr.append((lambda T4=T4, j=j: lambda: P.transpose(psK[:, j, :], dv["kT"][:, (T4 + j) * 128:(T4 + j + 1) * 128], ident[:]))())
            tr.append((lambda T4=T4, n8=n8: lambda: P.copy("act", km[:, T4:T4 + n8, :], psK[:, 0:n8, :]))())
        st = [lambda: P.memset("pool", Sin32[:, order[0], :], 0.0)]
        for i in range(0, nt, 4):
            grp = order[i:i + 4]
            for j, T_ in enumerate(grp):
                st.append((lambda j=j, T_=T_: lambda: P.mm(psU[:, j * 128:(j + 1) * 128], km[:, T_, :], v[:, T_, :]))())
            for j, T_ in enumerate(grp):
                idx = i + j
                if idx + 1 < nt:
                    Tn = order[idx + 1]
                    st.append((lambda j=j, T_=T_, Tn=Tn: lambda: P.stt(Sin32[:, Tn, :], Sin32[:, T_, :], a[:, T_:T_ + 1],
                                                                     psU[:, j * 128:(j + 1) * 128], ALU.mult, ALU.add))())
        st.append(lambda: P.copy("act", Sinbf[:], Sin32[:]))
        th += [S2, S1, S3, E2, E1, S4, M2, Ea, S5, M1, E4q, E4k]
        th += tr
        th += [S6, M4q, M4k, E5q, E5k]
        th += st
        th += [M5q, M5k, E6q, E6k, M6q, M6k]
        return ctx, th

    units = [(s, d, c) for s in range(nseq) for d in range(2) for c in range(2)]
    cur = make_prep(*units[0])
    for t_ in cur[1]:
        t_()
    for ui, (s, d, c) in enumerate(units):
        ctx = cur[0]
        v, a, km, dv, Sinbf = ctx["v"], ctx["a"], ctx["km"], ctx["dv"], ctx["Sinbf"]
        nxt_th = []
        if ui + 1 < len(units):
            cur = make_prep(*units[ui + 1])
            nxt_th = list(cur[1])
        per_tile = (len(nxt_th) + nt - 1) // nt if nxt_th else 0
        c0 = s * Ls
        order = list(range(nt)) if d == 0 else list(range(nt - 1, -1, -1))
        m2 = msk[:, d, :]
        kA, qA, kB, qB, kC, qC = dv["kA"], dv["qA"], dv["kB"], dv["qB"], dv["kC"], dv["qC"]

        def scores(T_):
            t0 = T_ * 128
            res = []
            for h in range(2):
                hr = slice(h * 64, (h + 1) * 64)
                pc = psC.next()
                P.mm(pc[:, 0:128], kC[hr, t0:t0 + 128], qC[hr, t0:t0 + 128])
                if d == 0:
                    P.mm(pc[0:64, 192:256], kA[hr, t0:t0 + 64], qA[hr, t0 + 64:t0 + 128])
                    P.mm(pc[0:32, 160:192], kB[hr, t0:t0 + 32], qB[hr, t0 + 32:t0 + 64])
                    P.mm(pc[64:96, 224:256], kB[hr, t0 + 64:t0 + 96], qB[hr, t0 + 96:t0 + 128])
                else:
                    P.mm(pc[64:128, 128:192], kA[hr, t0 + 64:t0 + 128], qA[hr, t0:t0 + 64])
                    P.mm(pc[0:64, 128:160], kB[hr, t0:t0 + 64], qB[hr, t0:t0 + 32])
                    P.mm(pc[64:128, 192:224], kB[hr, t0 + 64:t0 + 128], qB[hr, t0 + 64:t0 + 96])
                G = Gt[d].next()
                cpred(P, G[:], m2, pc[:, 0:256])
                res.append(G)
            return res

        nxt = scores(order[0])
        for oi, T_ in enumerate(order):
            t0 = T_ * 128
            Gs = nxt
            if oi + 1 < nt:
                nxt = scores(order[oi + 1])
            po = psO.next()
            for h in range(2):
                hr = slice(h * 64, (h + 1) * 64)
                oc = slice(h * 64, (h + 1) * 64)
                P.mm(po[:, oc], Gs[h][:, 0:128], v[:, T_, oc], start=True, stop=False)
                P.mm(po[:, oc], Gs[h][:, 128:256], v[:, T_, oc], start=False, stop=False)
                P.mm(po[:, oc], dv["qT"][hr, t0:t0 + 128], Sinbf[hr, T_, oc], start=False, stop=True)
            oa = oacc[:, T_, c * 128:(c + 1) * 128]
            if d == 0:
                P.copy("act", oa, po[:, 0:128])
            else:
                P.tt("dve", oa, oa, po[:, 0:128], ALU.add)
            for _ in range(per_tile):
                if nxt_th:
                    nxt_th.pop(0)()
        while nxt_th:
            nxt_th.pop(0)()
        if d == 1 and c == 1:
            for T_ in range(nt):
                P.dma("act", go[c0 + T_ * 128:c0 + (T_ + 1) * 128, :], oacc[:, T_, :])
    P.end()


def phase_MApre(P, nseq, pT, cw_d, cb_d, dtb_d, alog_d, selh_d, selg_d, gq, gk, gg, gv):
    P.begin("MApre")
    ident = make_ident(P)
    cw = P.sb("cw", [128, 4, 5], F32)
    cb = P.sb("cb", [128, 4], F32)
    dtb = P.sb("dtb", [8, 1], F32)
    aneg = P.sb("aneg", [8, 1], F32)
    selh = P.sb("selh", [8, 4, 128], F32)
    selg = P.sb("selg", [128, 2, 128], BF16)
    P.dma("sp", cw[:], cw_d[:])
    P.dma("sp", cb[:], cb_d[:])
    P.dma("sp", dtb[:], dtb_d[:])
    P.dma("sp", aneg[:], alog_d[:])
    P.dma("sp", selh[:], selh_d[:].rr("j d c m -> j (d c) m"))
    P.dma("sp", selg[:], selg_d[:])
    P.act(aneg[:], aneg[:], AF.Exp)
    P.ts("dve", aneg[:], aneg[:], -1.0, ALU.mult)
    Pp = Rot([P.sb("Pp", [128, L + 4], F32) for _ in range(2)])
    for t_ in Pp.t:
        P.memset("pool", t_[:, 0:2], 0.0)
        P.memset("pool", t_[:, L + 2:L + 4], 0.0)
    acc = Rot([P.sb("acc", [128, L], F32) for _ in range(2)])
    xbc = [P.sb("xbc%d" % c, [128, L], BF16) for c in range(4)]
    dtr = P.sb("dtr", [8, L], F32)
    dt = P.sb("dt", [8, L], F32)
    dta = P.sb("dta", [8, L], F32)
    vt = P.sb("vt", [128, NT, 256], BF16)
    rep = Rot([P.sb("rep", [128, 512], F32) for _ in range(2)])
    ob = Rot([P.sb("ob", [128, L], BF16) for _ in range(2)])
    og = Rot([P.sb("og", [128, L], F32) for _ in range(2)])
    psA = Rot([bank(P, "psA") for _ in range(2)])
    psB = Rot([bank(P, "psB") for _ in range(2)])
    psT = Rot([P.ps("psT", [128, 8, 128], BF16) for _ in range(2)])
    for s in range(nseq):
        c0 = s * L
        for c in range(4):
            pp = Pp.next()
            P.dma("sp", pp[:, 2:L + 2], pT[R_MX + c * 128:R_MX + (c + 1) * 128, c0:c0 + L])
            a = acc.next()
            P.ts("dve", a[:], pp[:, 0:L], cw[:, c, 0:1], ALU.mult)
            for j in range(1, 5):
                P.stt(a[:], pp[:, j:j + L], cw[:, c, j:j + 1], a[:], ALU.mult, ALU.add)
            P.act(xbc[c][:], a[:], AF.Silu, bias=cb[:, c:c + 1])
        P.dma("sp", dtr[:], pT[R_DT:R_DT + 8, c0:c0 + L])
        P.act(dt[:], dtr[:], AF.Exp, bias=dtb[:])
        P.act(dt[:], dt[:], AF.Ln, bias=1.0)
        P.ts("dve", dta[:], dt[:], aneg[:], ALU.mult)
        for T8 in range(0, NT, 4):
            pt = psT.next()
            for j in range(4):
                for c in range(2):
                    P.transpose(pt[:, j * 2 + c, :], xbc[c][:, (T8 + j) * 128:(T8 + j + 1) * 128], ident[:])
            P.copy("act", vt[:, T8:T8 + 4, :].rr("p n (c m) -> p (n c) m", c=2), pt[:])
        P.dma("act", gv[c0:c0 + L, :].rr("(n p) c -> p n c", p=128), vt[:])
        for c in range(2):
            oq = ob.next()
            for g4 in range(4):
                cs = slice(g4 * 512, (g4 + 1) * 512)
                pa = psA.next()
                P.mm(pa[:], selg[:, c, :], xbc[3][:, cs])
                P.copy("act", oq[:, cs], pa[:])
            P.dma("act", gq[c * 128:(c + 1) * 128, c0:c0 + L], oq[:])
            for d in range(2):
                ok_, og_ = ob.next(), og.next()
                for g4 in range(4):
                    cs = slice(g4 * 512, (g4 + 1) * 512)
                    pa = psA.next()
                    P.mm(pa[:], selg[:, c, :], xbc[2][:, cs])
                    r = rep.next()
                    P.copy("act", r[:], pa[:])
                    pb = psB.next()
                    P.mm(pb[:], selh[:, d * 2 + c, :], dt[:, cs])
                    P.tt("dve", ok_[:, cs], r[:], pb[:], ALU.mult)
                    pb2 = psB.next()
                    P.mm(pb2[:], selh[:, d * 2 + c, :], dta[:, cs])
                    P.copy("act", og_[:, cs], pb2[:])
                P.dma("act", gk[d, c * 128:(c + 1) * 128, c0:c0 + L], ok_[:])
                P.dma("act", gg[d, c * 128:(c + 1) * 128, c0:c0 + L], og_[:])
    P.end()


def tm_to_mixT(P, ident, psT, stage, yb, i4):
    pt = psT.next()
    for c in range(2):
        P.transpose(pt[:, c, :], yb[:, c * 128:(c + 1) * 128], ident[:])
    P.copy("act", stage[:, :, i4 * 128:(i4 + 1) * 128], pt[:, 0:2, :])


def phase_MApost(P, T, go, gv, ptm, dsk_d, nw_d, mixT):
    P.begin("MApost")
    ident = make_ident(P)
    dsk = P.sb("dsk", [128, 256], F32)
    nwt = P.sb("nwt", [128, 256], F32)
    P.dma("sp", dsk[:], dsk_d[:])
    P.dma("sp", nwt[:], nw_d[:])
    ob = Rot([P.sb("o", [128, 256], F32) for _ in range(4)])
    xb = Rot([P.sb("xs", [128, 256], BF16) for _ in range(4)])
    zb = Rot([P.sb("z", [128, 256], F32) for _ in range(4)])
    yb = Rot([P.sb("y", [128, 256], F32) for _ in range(4)])
    sq = P.sb("sq", [128, 256], F32)
    ssb = Rot([P.sb("ss", [128, 1], F32) for _ in range(4)])
    rsb = Rot([P.sb("rs", [128, 1], F32) for _ in range(4)])
    ybf = Rot([P.sb("ybf", [128, 256], BF16) for _ in range(4)])
    stg = Rot([P.sb("stg", [128, 2, 512], BF16) for _ in range(2)])
    psT = Rot([P.ps("psT", [128, 8, 128], BF16) for _ in range(2)])
    for g in range(T // 512):
        st = stg.next()
        for i4 in range(4):
            t0 = (g * 4 + i4) * 128
            o, xs, z, y = ob.next(), xb.next(), zb.next(), yb.next()
            P.dma("sp", o[:], go[t0:t0 + 128, :])
            P.dma("sp", xs[:], gv[t0:t0 + 128, :])
            P.dma("sp", z[:], ptm[t0:t0 + 128, C_MZ:C_MZ + 256])
            P.tt("dve", y[:], xs[:], dsk[:], ALU.mult)
            P.tt("dve", y[:], y[:], o[:], ALU.add)
            P.act(z[:], z[:], AF.Silu)
            P.tt("dve", y[:], y[:], z[:], ALU.mult)
            ss, rs = ssb.next(), rsb.next()
            P.act(sq[:], y[:], AF.Square, accum=ss[:])
            rms_rstd(P, ss, rs, 256)
            yo = ybf.next()
            P.stt(yo[:], y[:], rs[:], nwt[:], ALU.mult, ALU.mult)
            tm_to_mixT(P, ident, psT, st, yo, i4)
        P.dma("act", mixT[256:512, g * 512:(g + 1) * 512].rr("(c p) t -> p c t", p=128), st[:])
    P.end()


def phase_HGpre(P, nseq, layer, pT, ptm, lbraw_d, gq, gk, gg, gv):
    P.begin("HGpre")
    lbr = P.sb("lbr", [128, 2, 4], F32)
    P.dma("sp", lbr[:], lbraw_d[:].rr("p l d c -> p l (d c)"))
    lb = P.sb("lb", [128, 4], F32)
    oml = P.sb("oml", [128, 4], F32)
    s0 = P.sb("s0", [128, 4], F32)
    P.tt("dve", s0[:], lbr[:, 0, :], lbr[:, 1, :], ALU.subtract)
    P.act(s0[:], s0[:], AF.Sigmoid)
    if layer == 0:
        P.tt("dve", lb[:], s0[:], s0[:], ALU.subtract)
    else:
        s1 = P.sb("s1", [128, 4], F32)
        P.tt("dve", s1[:], lbr[:, 1, :], lbr[:, 0, :], ALU.subtract)
        P.act(s1[:], s1[:], AF.Sigmoid)
        P.tt("dve", lb[:], s0[:], s1[:], ALU.add)
        P.tt("dve", lb[:], lb[:], s0[:], ALU.subtract)
    P.ts("dve", oml[:], lb[:], -1.0, ALU.mult, 1.0, ALU.add)
    inb = Rot([P.sb("in", [128, L], F32) for _ in range(3)])
    sg = Rot([P.sb("sg", [128, L], F32) for _ in range(2)])
    ob = Rot([P.sb("ob", [128, L], BF16) for _ in range(3)])
    og = Rot([P.sb("og", [128, L], F32) for _ in range(2)])
    vi = Rot([P.sb("vi", [128, NT, 256], F32) for _ in range(1)])
    vo = Rot([P.sb("vo", [128, NT, 256], BF16) for _ in range(1)])
    for s in range(nseq):
        c0 = s * L
        for c in range(2):
            x = inb.next()
            P.dma("sp", x[:], pT[R_HG + c * 128:R_HG + (c + 1) * 128, c0:c0 + L])
            o = ob.next()
            P.act(o[:], x[:], AF.Silu)
            P.dma("pool", gq[c * 128:(c + 1) * 128, c0:c0 + L], o[:])
            for d in range(2):
                x = inb.next()
                r0 = R_HG + 256 + d * 256 + c * 128
                P.dma("sp", x[:], pT[r0:r0 + 128, c0:c0 + L])
                j = d * 2 + c
                sgm = sg.next()
                P.act(sgm[:], x[:], AF.Sigmoid)
                P.ts("dve", sgm[:], sgm[:], oml[:, j:j + 1], ALU.mult, lb[:, j:j + 1], ALU.add)
                g_ = og.next()
                P.act(g_[:], sgm[:], AF.Ln)
                P.dma("pool", gg[d, c * 128:(c + 1) * 128, c0:c0 + L], g_[:])
                sk = sg.next()
                P.act(sk[:], x[:], AF.Sigmoid, scale=-1.0)
                k_ = ob.next()
                P.ts("dve", k_[:], sk[:], oml[:, j:j + 1], ALU.mult)
                P.dma("pool", gk[d, c * 128:(c + 1) * 128, c0:c0 + L], k_[:])
        v_in, v_out = vi.next(), vo.next()
        P.dma("sp", v_in[:], ptm[c0:c0 + L, C_HI:C_HI + 256].rr("(n p) c -> p n c", p=128))
        P.copy("pool", v_out[:], v_in[:])
        P.dma("pool", gv[c0:c0 + L, :].rr("(n p) c -> p n c", p=128), v_out[:])
    P.end()


def phase_HGpost(P, T, go, ptm, nw_d, mixT):
    P.begin("HGpost")
    ident = make_ident(P)
    nwt = P.sb("nwt", [128, 256], F32)
    P.dma("sp", nwt[:], nw_d[:])
    ob = Rot([P.sb("o", [128, 256], F32) for _ in range(4)])
    zb = Rot([P.sb("z", [128, 256], F32) for _ in range(4)])
    sq = P.sb("sq", [128, 256], F32)
    ssb = Rot([P.sb("ss", [128, 4], F32) for _ in range(4)])
    ybf = Rot([P.sb("ybf", [128, 256], BF16) for _ in range(4)])
    stg = Rot([P.sb("stg", [128, 2, 512], BF16) for _ in range(2)])
    psT = Rot([P.ps("psT", [128, 8, 128], BF16) for _ in range(2)])
    for g in range(T // 512):
        st = stg.next()
        for i4 in range(4):
            t0 = (g * 4 + i4) * 128
            o, z = ob.next(), zb.next()
            P.dma("sp", o[:], go[t0:t0 + 128, :])
            P.dma("sp", z[:], ptm[t0:t0 + 128, C_HGATE:C_HGATE + 256])
            ss = ssb.next()
            P.tt("pool", sq[:], o[:], o[:], ALU.mult)
            P.reduce(ss[:], sq[:].rr("p (h e) -> p h e", e=64), ALU.add)
            P.ts("dve", ss[:], ss[:], 1.0 / 64, ALU.mult, EPS, ALU.add)
            P.act(ss[:], ss[:], AF.Sqrt)
            P.recip(ss[:], ss[:])
            P.tt("dve", o[:].rr("p (h e) -> p h e", e=64), o[:].rr("p (h e) -> p h e", e=64),
                 ss[:].unsq(2).bcast([128, 4, 64]), ALU.mult)
            P.act(z[:], z[:], AF.Silu)
            P.tt("dve", o[:], o[:], nwt[:], ALU.mult)
            yo = ybf.next()
            P.tt("dve", yo[:], o[:], z[:], ALU.mult)
            tm_to_mixT(P, ident, psT, st, yo, i4)
        P.dma("act", mixT[768:1024, g * 512:(g + 1) * 512].rr("(c p) t -> p c t", p=128), st[:])
    P.end()


def phase_ATE(P, rbrep_d, onehot_d, multrep_d, Mtab):
    P.begin("ATE")
    rb = P.sb("rb", [32, 4, 128], F32)
    oh = P.sb("oh", [32, 4096], F32)
    mu = P.sb("mu", [128, 4096], F32)
    P.dma("sp", rb[:], rbrep_d[:].rr("h b m -> b h m"))
    P.dma("sp", oh[:], onehot_d[:])
    P.dma("sp", mu[:], multrep_d[:])
    row = Rot([P.sb("row", [128, 4096], BF16) for _ in range(2)])
    eb = Rot([P.sb("eb", [128, 512], F32) for _ in range(2)])
    ps = Rot([bank(P, "ps") for _ in range(2)])
    for h in range(4):
        r = row.next()
        for ch in range(8):
            cs = slice(ch * 512, (ch + 1) * 512)
            p = ps.next()
            P.mm(p[:], rb[:, h, :], oh[:, cs])
            e = eb.next()
            P.act(e[:], p[:], AF.Exp)
            P.tt("dve", r[:, cs], e[:], mu[:, cs], ALU.mult)
        P.dma("sp", Mtab[h], r[:])
    P.end()


def phase_AT(P, nseq, pT, ptm, Mtab, selat_d, mixT):
    P.begin("AT")
    SW = 31 * 128
    strips = P.sb("strips", [128, 4, SW], BF16)
    for h in range(4):
        src = bass.AP(Mtab.h, h * 128 * 4096 + 127, [[4095, 128], [1, SW]])
        P.dma("sp", strips[:, h, :], Ref(src, Mtab, None))
    selat = P.sb("selat", [128, 2, 65], F32)
    P.dma("sp", selat[:], selat_d[:])
    qp = [P.sb("qp%d" % h, [65, L], BF16) for h in range(4)]
    kp = [P.sb("kp%d" % h, [65, L], BF16) for h in range(4)]
    for h in range(4):
        P.memset("pool", kp[h][64:65, :], 1.0)
    Vp = P.sb("Vp", [128, NT, 4, 128], BF16)
    P.memset("pool", Vp[:], 1.0)
    inb = Rot([P.sb("in", [128, L], F32) for _ in range(2)])
    sqb = Rot([P.sb("sqb", [128, L], F32) for _ in range(2)])
    vin = P.sb("vin", [128, NT, 256], F32)
    kmx = P.sb("kmx", [65, 4, 4], F32)
    kmax2 = P.sb("kmax2", [65, 4], F32)
    tmpr = Rot([P.sb("tmpr", [65, 512], F32) for _ in range(2)])
    Pt = Rot([P.sb("Pt", [128, 512], BF16) for _ in range(4)])
    PEt = Rot([P.sb("PEt", [128, 512], BF16) for _ in range(4)])
    rden = Rot([P.sb("rden", [128, 512], F32) for _ in range(2)])
    osb = P.sb("osb", [128, 2, L], BF16)
    psS = Rot([bank(P, "psS") for _ in range(4)])
    psO = Rot([bank(P, "psO") for _ in range(2)])
    psN = Rot([bank(P, "psN") for _ in range(2)])
    for s in range(nseq):
        c0 = s * L
        for (isq, cc) in ((0, 2), (0, 3), (1, 0), (1, 1)):
            x = inb.next()
            P.dma("sp", x[:], pT[R_AT + cc * 128:R_AT + (cc + 1) * 128, c0:c0 + L])
            c = cc % 2
            dst = qp if isq else kp
            sc = 0.125 if isq else 1.0
            P.act(dst[2 * c][0:64, :], x[0:64, :], AF.Copy, scale=sc)
            P.ts("dve", dst[2 * c + 1][0:64, :], x[64:128, :], sc, ALU.mult)
            sq = sqb.next()
            P.act(sq[:], x[:], AF.Square, scale=sc)
            for hh in range(2):
                h = 2 * c + hh
                for g in range(4):
                    cs = slice(g * 512, (g + 1) * 512)
                    pn = psN.next()
                    P.mm(pn[0:65, :], selat[:, hh, :], sq[:, cs])
                    if not isq:
                        P.reduce(kmx[:, h, g:g + 1], pn[0:65, :], ALU.max)
                    else:
                        tr = tmpr.next()
                        P.act(tr[64:65, :], pn[64:65, :], AF.Sqrt, scale=kmax2[64:65, h:h + 1])
                        P.ts("dve", qp[h][64:65, cs], tr[64:65, :], -1.0, ALU.mult)
                if not isq:
                    P.reduce(kmax2[:, h:h + 1], kmx[:, h, :], ALU.max)
                    P.ts("dve", kmax2[:, h:h + 1], kmax2[:, h:h + 1], 1.1, ALU.mult)
        P.dma("sp", vin[:], ptm[c0:c0 + L, C_AV:C_AV + 256].rr("(n p) c -> p n c", p=128))
        P.copy("pool", Vp[:, :, :, 0:64], vin[:].rr("p n (h e) -> p n h e", e=64))
        blocks = []
        for h in range(4):
            for g in range(4):
                js = [j for j in range(NT) if 4 * g - 8 <= j <= 4 * g + 11]
                for n_, j in enumerate(js):
                    blocks.append((h, g, j, n_ == 0, n_ == len(js) - 1))
        pend = []

        def issue_qk(b):
            h, g, j, _, _ = b
            p = psS.next()
            P.mm(p[:], kp[h][0:65, j * 128:(j + 1) * 128], qp[h][0:65, g * 512:(g + 1) * 512])
            pend.append(p)

        LOOK = 2
        for b in blocks[:LOOK]:
            issue_qk(b)
        po = None
        for bi, (h, g, j, first, last) in enumerate(blocks):
            if bi + LOOK < len(blocks):
                issue_qk(blocks[bi + LOOK])
            p = pend.pop(0)
            if first:
                po = psO.next()
            pt = Pt.next()
            P.act(pt[:], p[:], AF.Exp)
            pe = PEt.next()
            x0 = 128 * (4 * g - j + 15)
            P.tt("dve", pe[:], pt[:], strips[:, h, x0:x0 + 512], ALU.mult)
            P.mm(po[:], Vp[:, j, h, :], pe[:], start=first, stop=last)
            if last:
                rd = rden.next()
                P.recip(rd[64:128, :], po[64:128, :])
                hb = (h % 2) * 64
                P.tt("dve", osb[hb:hb + 64, h // 2, g * 512:(g + 1) * 512], po[0:64, :], rd[64:128, :], ALU.mult)
        P.dma("act", mixT[512:768, c0:c0 + L].rr("(c p) t -> p c t", p=128), osb[:])
    P.end()


TWO_PI = 2.0 * math.pi


def sin_layer(P, out, ps, freq, fb, tmp, m):
    P.ts("dve", tmp[:], ps, freq[:], ALU.mult, fb[:], ALU.add)
    P.ts("dve", m[:], tmp[:], math.pi, ALU.is_gt)
    P.stt(tmp[:], m[:], -TWO_PI, tmp[:], ALU.mult, ALU.add)
    P.ts("dve", m[:], tmp[:], -math.pi, ALU.is_lt)
    P.stt(tmp[:], m[:], TWO_PI, tmp[:], ALU.mult, ALU.add)
    P.act(out, tmp[:], AF.Sin)


def phase_HYF(P, zT_d, w1_d, w2_d, w3_d, pb_d, decf_d, decb_d, CTs_d, STs_d, Kd):
    P.begin("HYF")
    zT = P.sb("zT", [33, L], F32)
    w1 = P.sb("w1", [33, 64], F32)
    w2 = P.sb("w2", [64, 64], F32)
    w3 = P.sb("w3", [64, 512], F32)
    pb = P.sb("pb", [64, 3], F32)
    for (t_, d_) in ((zT, zT_d), (w1, w1_d), (w2, w2_d), (w3, w3_d), (pb, pb_d)):
        P.dma("sp", t_[:], d_[:])
    fb1 = P.sb("fb1", [64, 1], F32)
    fb2 = P.sb("fb2", [64, 1], F32)
    P.tt("dve", fb1[:], pb[:, 0:1], pb[:, 2:3], ALU.mult)
    P.tt("dve", fb2[:], pb[:, 1:2], pb[:, 2:3], ALU.mult)
    freq = pb[:, 2:3]
    h1 = P.sb("h1", [64, L], F32)
    h2 = P.sb("h2", [64, L], F32)
    tmp = Rot([P.sb("tmp", [64, 512], F32) for _ in range(2)])
    mm_ = Rot([P.sb("m", [64, 512], F32) for _ in range(2)])
    ps = Rot([bank(P, "ps") for _ in range(2)])
    for g in range(4):
        cs = slice(g * 512, (g + 1) * 512)
        p = ps.next()
        P.mm(p[0:64, :], w1[:], zT[:, cs])
        sin_layer(P, h1[:, cs], p[0:64, :], freq, fb1, tmp.next(), mm_.next())
    for g in range(4):
        cs = slice(g * 512, (g + 1) * 512)
        p = ps.next()
        P.mm(p[0:64, :], w2[:], h1[:, cs])
        sin_layer(P, h2[:, cs], p[0:64, :], freq, fb2, tmp.next(), mm_.next())
    hs = P.sb("hs", [128, NT, 256], BF16)
    hd = P.sb("hd", [128, NT, 256], BF16)
    dfb = Rot([P.sb("dfb", [128, 2, 256], F32) for _ in range(2)])
    hfb = Rot([P.sb("hfb", [128, 2, 256], F32) for _ in range(2)])
    for i in range(NT):
        df = dfb.next()
        P.dma("sp", df[:, 0, :], decf_d[i * 128:(i + 1) * 128, :])
        P.dma("sp", df[:, 1, :], decb_d[i * 128:(i + 1) * 128, :])
        p = ps.next()
        P.mm(p[:], h2[:, i * 128:(i + 1) * 128], w3[:])
        hf = hfb.next()
        P.tt("dve", hf[:], p[:].rr("p (a c) -> p a c", a=2), df[:], ALU.mult)
        P.tt("dve", hs[:, i, :], hf[:, 0, :], hf[:, 1, :], ALU.add)
        P.tt("dve", hd[:, i, :], hf[:, 1, :], hf[:, 0, :], ALU.subtract)
    ctb = Rot([P.sb("ctb", [128, NT, 128], BF16) for _ in range(2)])
    stb = Rot([P.sb("stb", [128, NT, 128], BF16) for _ in range(2)])
    kb = Rot([P.sb("kb", [128, 512], F32) for _ in range(2)])
    for j in range(16):
        ct, st = ctb.next(), stb.next()
        P.dma("sp", ct[:], CTs_d[j])
        P.dma("sp", st[:], STs_d[j])
        p = ps.next()
        for i in range(NT):
            P.mm(p[:, 0:256], ct[:, i, :], hs[:, i, :], start=(i == 0), stop=(i == NT - 1))
        for i in range(NT):
            P.mm(p[:, 256:512], st[:, i, :], hd[:, i, :], start=(i == 0), stop=(i == NT - 1))
        k_ = kb.next()
        P.copy("act", k_[:], p[:])
        P.dma("act", Kd[j], k_[:])
    P.end()


def phase_HY(P, nseq, pT, cw_d, fbias_d, Kd, CTs_d, STs_d, Ci_d, Si_d, mixT):
    P.begin("HY")
    ident = make_ident(P)
    cw = P.sb("cw", [128, 6, 3], F32)
    fbias = P.sb("fbias", [128, 2], F32)
    P.dma("sp", cw[:], cw_d[:])
    P.dma("sp", fbias[:], fbias_d[:])
    K = P.sb("K", [128, 16, 512], F32)
    P.dma("sp", K[:], Kd[:].rr("j p c -> p j c"))
    Pp = Rot([P.sb("Pp", [128, L + 2], F32) for _ in range(2)])
    for t_ in Pp.t:
        P.memset("pool", t_[:, 0:1], 0.0)
        P.memset("pool", t_[:, L + 1:L + 2], 0.0)
    x0 = P.sb("x0", [128, 2, L], F32)
    x1 = Rot([P.sb("x1", [128, L], F32) for _ in range(2)])
    vv = Rot([P.sb("vv", [128, L], F32) for _ in range(2)])
    wf = P.sb("wf", [128, 2, L], F32)
    wb = Rot([P.sb("wb", [128, L], BF16) for _ in range(2)])
    wT = P.sb("wT", [128, NT, 256], BF16)
    Y = P.sb("Y", [128, 16, 512], BF16)
    ctb = Rot([P.sb("ctb", [128, NT, 128], BF16) for _ in range(2)])
    stb = Rot([P.sb("stb", [128, NT, 128], BF16) for _ in range(2)])
    cib = Rot([P.sb("cib", [128, 1024], BF16) for _ in range(2)])
    sib = Rot([P.sb("sib", [128, 1024], BF16) for _ in range(2)])
    usb = Rot([P.sb("usb", [128, 512], F32) for _ in range(2)])
    t1 = Rot([P.sb("t1", [128, 256], F32) for _ in range(2)])
    t2 = Rot([P.sb("t2", [128, 256], F32) for _ in range(2)])
    ot = Rot([P.sb("ot", [128, 512], F32) for _ in range(2)])
    outb = P.sb("outb", [128, 2, L], BF16)
    psT = P.ps("psT", [128, 8, 128], BF16)
    psU = Rot([bank(P, "psU") for _ in range(2)])
    psI = [bank(P, "psI%d" % i) for i in range(4)]

    def conv3(dst, c, s):
        pp = Pp.next()
        P.dma("sp", pp[:, 1:L + 1], pT[R_HY + c * 128:R_HY + (c + 1) * 128, s * L:(s + 1) * L])
        P.ts("dve", dst, pp[:, 0:L], cw[:, c, 0:1], ALU.mult)
        P.stt(dst, pp[:, 1:L + 1], cw[:, c, 1:2], dst, ALU.mult, ALU.add)
        P.stt(dst, pp[:, 2:L + 2], cw[:, c, 2:3], dst, ALU.mult, ALU.add)

    for s in range(nseq):
        for c in range(2):
            conv3(x0[:, c, :], c, s)
            a, b = x1.next(), vv.next()
            conv3(a[:], 2 + c, s)
            conv3(b[:], 4 + c, s)
            P.tt("pool", wf[:, c, :], a[:], b[:], ALU.mult)
            w_ = wb.next()
            P.copy("act", w_[:], wf[:, c, :])
            for T8 in range(0, NT, 8):
                for j in range(8):
                    P.transpose(psT[:, j, :], w_[:, (T8 + j) * 128:(T8 + j + 1) * 128], ident[:])
                P.copy("act", wT[:, T8:T8 + 8, c * 128:(c + 1) * 128], psT[:])
        for j in range(16):
            ct, st = ctb.next(), stb.next()
            P.dma("sp", ct[:], CTs_d[j])
            P.dma("sp", st[:], STs_d[j])
            p = psU.next()
            for i in range(NT):
                P.mm(p[:, 0:256], ct[:, i, :], wT[:, i, :], start=(i == 0), stop=(i == NT - 1))
            for i in range(NT):
                P.mm(p[:, 256:512], st[:, i, :], wT[:, i, :], start=(i == 0), stop=(i == NT - 1))
            u = usb.next()
            P.copy("act", u[:], p[:])
            ur, us = u[:, 0:256], u[:, 256:512]
            kr, ki = K[:, j, 0:256], K[:, j, 256:512]
            a, b = t1.next(), t2.next()
            P.tt("dve", a[:], ur, kr, ALU.mult)
            P.tt("pool", b[:], us, ki, ALU.mult)
            P.tt("dve", Y[:, j, 0:256], a[:], b[:], ALU.add)
            a, b = t1.next(), t2.next()
            P.tt("dve", a[:], us, kr, ALU.mult)
            P.tt("pool", b[:], ur, ki, ALU.mult)
            P.tt("dve", Y[:, j, 256:512], a[:], b[:], ALU.subtract)
        for nh in range(2):
            for j in range(16):
                ci, si = cib.next(), sib.next()
                P.dma("sp", ci[:], Ci_d[j, :, nh * 1024:(nh + 1) * 1024])
                P.dma("sp", si[:], Si_d[j, :, nh * 1024:(nh + 1) * 1024])
                for cc in range(2):
                    for ng in range(2):
                        p = psI[cc * 2 + ng]
                        P.mm(p[:], Y[:, j, cc * 128:(cc + 1) * 128], ci[:, ng * 512:(ng + 1) * 512],
                             start=(j == 0), stop=False)
                        P.mm(p[:], Y[:, j, 256 + cc * 128:256 + (cc + 1) * 128], si[:, ng * 512:(ng + 1) * 512],
                             start=False, stop=(j == 15))
            for cc in range(2):
                for ng in range(2):
                    ns = slice(nh * 1024 + ng * 512, nh * 1024 + (ng + 1) * 512)
                    o = ot.next()
                    P.stt(o[:], wf[:, cc, ns], fbias[:, cc:cc + 1], psI[cc * 2 + ng][:], ALU.mult, ALU.add)
                    P.tt("dve", outb[:, cc, ns], o[:], x0[:, cc, ns], ALU.mult)
        P.dma("act", mixT[0:256, s * L:(s + 1) * L].rr("(c p) t -> p c t", p=128), outb[:])
    P.end()


def phase_O(P, nseq, mixT, w_out, Xin, Xout, hn2, LG, wr_d, nw2_d):
    P.begin("O")
    identf = make_ident(P, F32)
    Wo = P.sb("Wo", [128, 8, D], BF16)
    stg = Rot([P.sb("stg", [128, D], F32) for _ in range(2)])
    for k in range(8):
        st = stg.next()
        P.dma("sp", st[:], w_out[k * 128:(k + 1) * 128, :])
        P.copy(("pool", "dve", "act")[k % 3], Wo[:, k, :], st[:])
    nw2 = P.sb("nw2", [128, 8], F32)
    Wr = P.sb("Wr", [128, 8, 16], F32)
    P.dma("sp", nw2[:], nw2_d[:])
    P.dma("sp", Wr[:], wr_d[:])
    P.tt("dve", Wr[:], Wr[:], nw2[:].unsq(2).bcast([128, 8, 16]), ALU.mult)
    mTb = Rot([P.sb("mT", [128, 8, 512], BF16) for _ in range(2)])
    xb = Rot([P.sb("x", [128, D], F32) for _ in range(2)])
    x1b = Rot([P.sb("x1", [128, D], F32) for _ in range(2)])
    sq = P.sb("sq", [128, D], F32)
    ssb = Rot([P.sb("ss", [128, 1], F32) for _ in range(2)])
    rsb = Rot([P.sb("rs", [128, 1], F32) for _ in range(2)])
    hnf = Rot([P.sb("hnf", [128, D], F32) for _ in range(3)])
    hnb = Rot([P.sb("hnb", [128, D], BF16) for _ in range(2)])
    hT = Rot([P.sb("hT", [128, 8, 128], F32) for _ in range(2)])
    lgrow = Rot([P.sb("lgrow", [16, L], F32) for _ in range(2)])
    psO = Rot([bank(P, "psO") for _ in range(4)])
    psT = Rot([bank(P, "psT") for _ in range(2)])
    psR = bank(P, "psR")
    mview = mixT[:].rr("(k p) t -> p k t", p=128)
    tiles = [(s_, g, i4) for s_ in range(nseq) for g in range(L // 512) for i4 in range(4)]
    mts = {}
    lgs = {}

    def stageA(s_, g, i4):
        tg = s_ * L + g * 512
        if i4 == 0:
            mts[(s_, g)] = mTb.next()
            P.dma("sp", mts[(s_, g)][:], mview[:, :, tg:tg + 512])
        mT = mts[(s_, g)]
        t0 = tg + i4 * 128
        x, x1 = xb.next(), x1b.next()
        P.dma("sp", x[:], Xin[t0:t0 + 128, :])
        for half in range(2):
            p = psO.next()
            for k in range(8):
                P.mm(p[:], mT[:, k, i4 * 128:(i4 + 1) * 128], Wo[:, k, half * 512:(half + 1) * 512],
                     start=(k == 0), stop=(k == 7))
            P.tt("dve", x1[:, half * 512:(half + 1) * 512], x[:, half * 512:(half + 1) * 512], p[:], ALU.add)
        P.dma("pool", Xout[t0:t0 + 128, :], x1[:])
        ss, rs = ssb.next(), rsb.next()
        P.act(sq[:], x1[:], AF.Square, accum=ss[:])
        rms_rstd(P, ss, rs, D)
        hf, hb = hnf.next(), hnb.next()
        P.ts("dve", hf[:], x1[:], rs[:], ALU.mult)
        P.copy("pool", hb[:], hf[:])
        P.dma("pool", hn2[t0:t0 + 128, :], hb[:])
        return hf

    def stageB(s_, g, i4, hf):
        if g == 0 and i4 == 0:
            lgs[s_] = lgrow.next()
        lg = lgs[s_]
        h_ = hT.next()
        for k4 in range(2):
            pt = psT.next()
            for k in range(4):
                P.transpose(pt[:, k * 128:(k + 1) * 128], hf[:, (k4 * 4 + k) * 128:(k4 * 4 + k + 1) * 128], identf[:])
            P.copy("act", h_[:, k4 * 4:(k4 + 1) * 4, :], pt[:].rr("p (k t) -> p k t", k=4))
        for k in range(8):
            P.mm(psR[0:16, 0:128], Wr[:, k, :], h_[:, k, :], start=(k == 0), stop=(k == 7))
        tl = g * 512 + i4 * 128
        P.copy("act", lg[:, tl:tl + 128], psR[0:16, 0:128])
        if g == L // 512 - 1 and i4 == 3:
            P.dma("pool", LG[s_ * 16:(s_ + 1) * 16, :], lg[:])

    hf_next = stageA(*tiles[0])
    for ti, tl_ in enumerate(tiles):
        hf_cur = hf_next
        if ti + 1 < len(tiles):
            hf_next = stageA(*tiles[ti + 1])
        stageB(*tl_, hf_cur)
    P.end()


def phase_R(P, nseq, LG, blk_d, offs_d, GT, IT):
    P.begin("R")
    R_ = nseq * 16
    identf = make_ident(P, F32)
    lg = P.sb("lg", [R_, L], F32)
    P.dma("sp", lg[:], LG[:])
    blk = P.sb("blk", [64, 64], F32)
    offs = P.sb("offs", [64, 1], F32)
    P.dma("sp", blk[:], blk_d[:])
    P.dma("sp", offs[:], offs_d[:])
    P.ts("dve", lg[:], lg[:], 80.0, ALU.min)
    e = P.sb("e", [R_, L], F32)
    P.act(e[:], lg[:], AF.Exp)
    aff = P.sb("aff", [R_, L], F32)
    rd = P.sb("rd", [R_, 512], F32)
    ps = Rot([bank(P, "ps") for _ in range(2)])
    for g in range(4):
        cs = slice(g * 512, (g + 1) * 512)
        p = ps.next()
        P.mm(p[0:R_, :], blk[0:R_, 0:R_], e[:, cs])
        P.recip(rd[:], p[0:R_, :])
        P.tt("dve", aff[:, cs], e[:, cs], rd[:], ALU.mult)
    gates = P.sb("gates", [R_, 256], F32)
    idx = P.sb("idx", [R_, 256], U32)
    for it in range(32):
        m8 = gates[:, it * 8:(it + 1) * 8]
        i8 = idx[:, it * 8:(it + 1) * 8]
        P.generic("dve", (lambda m8=m8: lambda en: en.max(m8.ap, aff.h[:, :]))(), [aff[:]], [m8])
        P.generic("dve", (lambda m8=m8, i8=i8: lambda en: en.max_index(i8.ap, m8.ap, aff.h[:, :]))(), [aff[:], m8], [i8])
        P.generic("dve", (lambda m8=m8: lambda en: en.match_replace(aff.h[:, :], m8.ap, aff.h[:, :], -1.0))(),
                  [aff[:], m8], [aff[:]])
    idf = P.sb("idf", [R_, 256], F32)
    P.copy("dve", idf[:], idx[:])
    P.ts("dve", idf[:], idf[:], offs[0:R_, :], ALU.add)
    gts = P.sb("gts", [128, 2, 64], F32)
    its = P.sb("its", [128, 2, 64], I32)
    P.memset("pool", gts[:], 0.0)
    P.memset("pool", its[:], 0)
    for j2 in range(2):
        p = ps.next()
        P.transpose(p[:, 0:R_], gates[:, j2 * 128:(j2 + 1) * 128], identf[0:R_, 0:R_])
        P.copy("act", gts[:, j2, 0:R_], p[:, 0:R_])
        p = ps.next()
        P.transpose(p[:, 0:R_], idf[:, j2 * 128:(j2 + 1) * 128], identf[0:R_, 0:R_])
        P.copy("dve", its[:, j2, 0:R_], p[:, 0:R_])
    P.dma("sp", GT[:], gts[:])
    P.dma("sp", IT[:], its[:])
    P.end()


def phase_E(P, nseq, wg_d, wu_d, wd_d, nw2_d, hn2, GT, IT, Xout, experts=range(16)):
    P.begin("E")
    identb = make_ident(P)
    gT = P.sb("gT", [128, 2, 64], F32)
    iT = P.sb("iT", [128, 2, 64], I32)
    nw2 = P.sb("nw2", [128, 8], F32)
    P.dma("sp", gT[:], GT[:])
    P.dma("sp", iT[:], IT[:])
    P.dma("sp", nw2[:], nw2_d[:])
    NSL = nseq * 256
    Wg = Rot([P.sb("Wg", [128, 8, D], BF16) for _ in range(2)])
    Wu = Rot([P.sb("Wu", [128, 8, D], BF16) for _ in range(2)])
    Wd = Rot([P.sb("Wd", [128, 8, D], BF16) for _ in range(2)])
    stg = Rot([P.sb("stg", [128, D], F32) for _ in range(6)])
    xeb = Rot([P.sb("xe", [128, D], BF16) for _ in range(3)])
    xeTb = Rot([P.sb("xeT", [128, 8, NSL], BF16) for _ in range(2)])
    hT = P.sb("hT", [128, 8, NSL], BF16)
    sil = Rot([P.sb("sil", [128, 512], F32) for _ in range(2)])
    yeb = Rot([P.sb("ye", [128, D], F32) for _ in range(2)])
    psT = Rot([P.ps("psT", [128, 8, 128], BF16) for _ in range(2)])
    psG = Rot([bank(P, "psG") for _ in range(2)])
    psU = Rot([bank(P, "psU") for _ in range(2)])
    psY = Rot([bank(P, "psY") for _ in range(2)])
    IOA = bass.IndirectOffsetOnAxis
    experts = list(experts)

    def prep_w(e):
        wg, wu, wd = Wg.next(), Wu.next(), Wd.next()
        groups = []
        for k in range(8):
            def grp(k=k):
                st = stg.next()
                P.dma("sp", st[:], wg_d[e, k * 128:(k + 1) * 128, :])
                P.ts("dve", wg[:, k, :], st[:], nw2[:, k:k + 1], ALU.mult)
                st = stg.next()
                P.dma("sp", st[:], wu_d[e, k * 128:(k + 1) * 128, :])
                P.act(wu[:, k, :], st[:], AF.Copy, scale=nw2[:, k:k + 1])
                st = stg.next()
                P.dma("sp", st[:], wd_d[e, k * 128:(k + 1) * 128, :])
                P.copy("dve" if k % 2 == 0 else "act", wd[:, k, :], st[:])
            groups.append(grp)
        return (wg, wu, wd), groups

    def prep_x(e):
        xeT = xeTb.next()
        for s in range(nseq):
            col = s * 16 + e
            for j2 in range(2):
                xe = xeb.next()
                ix = iT[:, j2, col:col + 1]
                P.dma_generic("pool", (lambda xe=xe, ix=ix: lambda en: en.indirect_dma_start(
                    out=xe.h[:, :], out_offset=None, in_=hn2.h.ap()[:, :], in_offset=IOA(ap=ix.ap, axis=0)))(),
                    [ix, hn2[:]], [xe[:]])
                pt = psT.next()
                for k in range(8):
                    P.transpose(pt[:, k, :], xe[:, k * 128:(k + 1) * 128], identb[:])
                sl0 = (s * 2 + j2) * 128
                P.copy("act" if j2 == 0 else "dve", xeT[:, :, sl0:sl0 + 128], pt[:])
        return xeT

    wcur, groups = prep_w(experts[0])
    for g_ in groups:
        g_()
    xcur = prep_x(experts[0])
    for ei, e in enumerate(experts):
        wg, wu, wd = wcur
        xeT = xcur
        groups = []
        if ei + 1 < len(experts):
            wcur, groups = prep_w(experts[ei + 1])
        for fc in range(8):
            for sg in range(0, NSL, 512):
                n = min(512, NSL - sg)
                pg, pu = psG.next(), psU.next()
                for k in range(8):
                    P.mm(pg[:, 0:n], wg[:, k, fc * 128:(fc + 1) * 128], xeT[:, k, sg:sg + n], start=(k == 0), stop=(k == 7))
                for k in range(8):
                    P.mm(pu[:, 0:n], wu[:, k, fc * 128:(fc + 1) * 128], xeT[:, k, sg:sg + n], start=(k == 0), stop=(k == 7))
                sl = sil.next()
                P.act(sl[:, 0:n], pg[:, 0:n], AF.Silu)
                P.tt("dve", hT[:, fc, sg:sg + n], sl[:, 0:n], pu[:, 0:n], ALU.mult)
            if groups:
                groups.pop(0)()
        if ei + 1 < len(experts):
            xcur = prep_x(experts[ei + 1])
        for s in range(nseq):
            col = s * 16 + e
            for j2 in range(2):
                sl0 = (s * 2 + j2) * 128
                ye = yeb.next()
                for half in range(2):
                    py = psY.next()
                    for fc in range(8):
                        P.mm(py[:], hT[:, fc, sl0:sl0 + 128], wd[:, fc, half * 512:(half + 1) * 512],
                             start=(fc == 0), stop=(fc == 7))
                    if half == 0:
                        P.act(ye[:, 0:512], py[:], AF.Copy, scale=gT[:, j2, col:col + 1])
                    else:
                        P.ts("dve", ye[:, 512:1024], py[:], gT[:, j2, col:col + 1], ALU.mult)
                ix = iT[:, j2, col:col + 1]
                P.dma_generic("pool", (lambda ye=ye, ix=ix: lambda en: en.indirect_dma_start(
                    out=Xout.h.ap()[:, :], out_offset=IOA(ap=ix.ap, axis=0), in_=ye.h[:, :], in_offset=None,
                    compute_op=ALU.add, oob_is_err=True))(),
                    [ix, ye[:]], [Xout[:]])
    P.end()


def phase_F(P, T, Xin, fw_d, out):
    P.begin("F")
    fw = P.sb("fw", [128, D], F32)
    P.dma("sp", fw[:], fw_d[:])
    xb = Rot([P.sb("x", [128, D], F32) for _ in range(3)])
    ob = Rot([P.sb("o", [128, D], F32) for _ in range(3)])
    sq = P.sb("sq", [128, D], F32)
    ssb = Rot([P.sb("ss", [128, 1], F32) for _ in range(2)])
    rsb = Rot([P.sb("rs", [128, 1], F32) for _ in range(2)])
    for i in range(T // 128):
        x, o = xb.next(), ob.next()
        P.dma("sp", x[:], Xin[i * 128:(i + 1) * 128, :])
        ss, rs = ssb.next(), rsb.next()
        P.act(sq[:], x[:], AF.Square, accum=ss[:])
        rms_rstd(P, ss, rs, D)
        P.stt(o[:], x[:], rs[:], fw[:], ALU.mult, ALU.mult)
        P.dma("pool", out[i * 128:(i + 1) * 128, :], o[:])
    P.end()


BF = ml_dtypes.bfloat16
_CONST_CACHE = {}


def host_consts():
    if _CONST_CACHE:
        return _CONST_CACHE
    c = {}
    c["masks"] = gla_masks()
    selh = np.zeros((8, 2, 2, 128), np.float32)
    for d in range(2):
        for cc in range(2):
            for m in range(128):
                selh[d * 4 + 2 * cc + m // 64, d, cc, m] = 1.0
    c["selh"] = selh
    selg = np.zeros((128, 2, 128), np.float32)
    for cc in range(2):
        for m in range(128):
            selg[cc * 64 + m % 64, cc, m] = 1.0
    c["selg"] = selg.astype(BF)
    selat = np.zeros((128, 2, 65), np.float32)
    selat[0:64, 0, :] = 1.0
    selat[64:128, 1, :] = 1.0
    c["selat"] = selat
    z = np.arange(4096)
    rel = (2047 - z).astype(np.int64)
    nb, max_exact = 16, 8
    ret = (rel > 0).astype(np.int32) * nb
    n = np.abs(rel)
    nf = np.maximum(n, 1).astype(np.float32)
    large = max_exact + (np.log(nf / np.float32(max_exact)) / np.float32(math.log(1024 / max_exact))
                         * np.float32(nb - max_exact)).astype(np.int32)
    large = np.minimum(large, nb - 1)
    bucket = ret + np.where(n < max_exact, n, large)
    oh = np.zeros((32, 4096), np.float32)
    oh[bucket, z] = 1.0
    c["onehot"] = oh
    mult = ((n <= 64).astype(np.float32) + ((n % 4 == 0) & (n <= 256)).astype(np.float32)
            + ((n % 16 == 0) & (n <= 1024)).astype(np.float32))
    c["multrep"] = np.ascontiguousarray(np.broadcast_to(mult[None, :], (128, 4096))).astype(np.float32)
    t = np.linspace(0.0, 1.0, L, dtype=np.float32)[:, None]
    ang_pos = (np.float32(2.0 * math.pi) * np.arange(L, dtype=np.float32) / np.float32(L)).astype(np.float32)
    f = np.linspace(1e-4, 15, 16, dtype=np.float32)
    ang = (ang_pos[:, None] * f[None, :]).astype(np.float32)
    zf = np.concatenate([t, np.cos(ang), -np.sin(ang)], axis=-1).astype(np.float32)
    c["zT"] = np.ascontiguousarray(zf.T)
    max_decay = math.log(1e-2) / 0.3
    min_decay = math.log(1e-2) / 1.5
    deltas = np.abs(np.linspace(min_decay, max_decay, 256, dtype=np.float32))
    dec = np.exp(-t * deltas[None, :]).astype(np.float32)
    c["decf"] = dec
    decb = dec.copy()
    decb[0] = 0.0
    c["decb"] = decb
    ff = np.arange(2048, dtype=np.float64)
    th = np.pi * (2 * ff[:, None] + 1) * ff[None, :] / 4096.0
    C, S = np.cos(th), np.sin(th)
    c["CTs"] = np.ascontiguousarray(C.reshape(16, 128, 16, 128).transpose(0, 3, 2, 1)).astype(BF)
    c["STs"] = np.ascontiguousarray(S.reshape(16, 128, 16, 128).transpose(0, 3, 2, 1)).astype(BF)
    c["Ci"] = (C * (2.0 / 4096.0)).reshape(16, 128, 2048).astype(BF)
    c["Si"] = (S * (2.0 / 4096.0)).reshape(16, 128, 2048).astype(BF)
    blk = np.zeros((64, 64), np.float32)
    for a in range(4):
        blk[a * 16:(a + 1) * 16, a * 16:(a + 1) * 16] = 1.0
    c["blk"] = blk
    c["offs"] = ((np.arange(64) // 16) * 2048).astype(np.float32)[:, None]
    _CONST_CACHE.update(c)
    return c


CONST_SPECS = [("masks", [2, 128, 256], U32), ("selh", [8, 2, 2, 128], F32), ("selg", [128, 2, 128], BF16),
               ("selat", [128, 2, 65], F32), ("onehot", [32, 4096], F32), ("multrep", [128, 4096], F32),
               ("zT", [33, L], F32), ("decf", [L, 256], F32), ("decb", [L, 256], F32),
               ("CTs", [16, 128, 16, 128], BF16), ("STs", [16, 128, 16, 128], BF16),
               ("Ci", [16, 128, 2048], BF16), ("Si", [16, 128, 2048], BF16),
               ("blk", [64, 64], F32), ("offs", [64, 1], F32)]


def pk(v):
    return np.ascontiguousarray(np.asarray(v).reshape(-1, 128).T)


def layer_params(inp, l):
    d = {}
    d["nw1"] = pk(inp["norm_mix_w"][l])
    d["nw2"] = pk(inp["norm_ffn_w"][l])
    d["hy_cw"] = np.ascontiguousarray(inp["hy_conv_w"][l].reshape(3, 6, 128).transpose(2, 1, 0))
    d["hy_w1"] = np.ascontiguousarray(inp["hy_pos_w1"][l])
    d["hy_w2"] = np.ascontiguousarray(inp["hy_pos_w2"][l])
    d["hy_w3"] = np.ascontiguousarray(inp["hy_pos_w3"][l])
    d["hy_pb"] = np.ascontiguousarray(np.stack([inp["hy_pos_b1"][l], inp["hy_pos_b2"][l], inp["hy_sin_freq"][l]], 1))
    d["hy_fb"] = np.ascontiguousarray(inp["hy_filt_bias"][l].reshape(2, 128).T)
    d["m_cw"] = np.ascontiguousarray(inp["m_conv_w"][l].reshape(5, 4, 128).transpose(2, 1, 0))
    d["m_cb"] = np.ascontiguousarray(inp["m_conv_b"][l].reshape(4, 128).T)
    d["m_dtb"] = np.ascontiguousarray(inp["m_dt_bias"][l].reshape(8, 1))
    d["m_alog"] = np.ascontiguousarray(inp["m_A_log"][l].reshape(8, 1))
    d["m_dsk"] = np.ascontiguousarray(np.broadcast_to(np.repeat(inp["m_D"][l], 64)[None, :], (128, 256)))
    d["m_nw"] = np.ascontiguousarray(np.broadcast_to(inp["m_norm_w"][l][None, :], (128, 256)))
    d["hg_nw"] = np.ascontiguousarray(np.broadcast_to(np.tile(inp["hg_norm_w"][l], 4)[None, :], (128, 256)))
    d["wr"] = np.ascontiguousarray(inp["router_w"][l].reshape(8, 128, 16).transpose(1, 0, 2))
    return {k: np.asarray(v, np.float32) for k, v in d.items()}


LAYER_SPECS = [("nw1", [128, 8]), ("nw2", [128, 8]), ("hy_cw", [128, 6, 3]), ("hy_w1", [33, 64]), ("hy_w2", [64, 64]),
               ("hy_w3", [64, 512]), ("hy_pb", [64, 3]), ("hy_fb", [128, 2]), ("m_cw", [128, 4, 5]), ("m_cb", [128, 4]),
               ("m_dtb", [8, 1]), ("m_alog", [8, 1]), ("m_dsk", [128, 256]), ("m_nw", [128, 256]), ("hg_nw", [128, 256]),
               ("wr", [128, 8, 16])]


def global_params(inp):
    d = {}
    d["rbrep"] = np.ascontiguousarray(np.broadcast_to(inp["rel_bias"].T[:, :, None], (4, 32, 128))).astype(np.float32)
    d["lbraw"] = np.ascontiguousarray(inp["hg_lb"].reshape(2, 2, 2, 128).transpose(3, 0, 1, 2)).astype(np.float32)
    d["fw"] = np.ascontiguousarray(np.broadcast_to(inp["final_norm_w"][None, :], (128, 1024))).astype(np.float32)
    return d


GLOBAL_SPECS = [("rbrep", [4, 32, 128]), ("lbraw", [128, 2, 2, 2]), ("fw", [128, 1024])]
DEPTH = 2


def build_program(nseq, depth=DEPTH, do_moe=True, do_mix=True, debug=None, experts=range(16)):
    T = nseq * L
    P = Prog()
    I = {}

    def inp(name, shape, dt):
        I[name] = P.dram(name, shape, dt, kind="ExternalInput")
        return I[name]

    x = inp("x", [T, D], F32)
    w_in = inp("w_in", [depth, D, 3592], F32)
    w_out = inp("w_out", [depth, D, D], F32)
    if do_moe:
        wg = inp("moe_w_gate", [depth, 16, D, D], F32)
        wu = inp("moe_w_up", [depth, 16, D, D], F32)
        wd = inp("moe_w_down", [depth, 16, D, D], F32)
    for (n, shp, dt) in CONST_SPECS:
        inp("c_" + n, shp, dt)
    for (n, shp) in GLOBAL_SPECS:
        inp("g_" + n, shp, F32)
    for l in range(depth):
        for (n, shp) in LAYER_SPECS:
            inp("l%d_%s" % (l, n), shp, F32)
    out = P.dram("out", [T, D], F32, kind="ExternalOutput")
    dbg = {}
    if debug:
        for (n, shp, dt) in debug:
            dbg[n] = P.dram("dbg_" + n, shp, dt, kind="ExternalOutput")

    def scratch(name, shape, dt):
        if name in dbg:
            return dbg[name]
        return P.dram("s_" + name, shape, dt)

    X = [x] + [scratch("X%d" % (l + 1), [T, D], F32) for l in range(depth)]
    pT = scratch("pT", [NFM, T], F32)
    ptm = scratch("ptm", [T, NTM], F32)
    mixT = scratch("mixT", [D, T], BF16) if do_mix else inp("mixT_in", [D, T], BF16)
    gq = scratch("gq", [256, T], BF16)
    gk = scratch("gk", [2, 256, T], BF16)
    gg = scratch("gg", [2, 256, T], F32)
    gv = scratch("gv", [T, 256], BF16)
    go = scratch("go", [T, 256], F32)
    Kd = scratch("Kd", [16, 128, 512], F32)
    Mtab = scratch("Mtab", [4, 128, 4096], BF16)
    hn2 = scratch("hn2", [T, D], BF16)
    LG = scratch("LG", [nseq * 16, L], F32)
    GT = scratch("GT", [128, 2, 64], F32)
    IT = scratch("IT", [128, 2, 64], I32)
    C = lambda n: I["c_" + n]
    if do_mix:
        phase_ATE(P, I["g_rbrep"], C("onehot"), C("multrep"), Mtab)
    for l in range(depth):
        Lp = lambda n: I["l%d_%s" % (l, n)]
        if do_mix:
            phase_P(P, T, X[l], w_in[l], Lp("nw1"), pT, ptm)
            phase_HYF(P, C("zT"), Lp("hy_w1"), Lp("hy_w2"), Lp("hy_w3"), Lp("hy_pb"), C("decf"), C("decb"),
                      C("CTs"), C("STs"), Kd)
            phase_HY(P, nseq, pT, Lp("hy_cw"), Lp("hy_fb"), Kd, C("CTs"), C("STs"), C("Ci"), C("Si"), mixT)
            phase_AT(P, nseq, pT, ptm, Mtab, C("selat"), mixT)
            phase_MApre(P, nseq, pT, Lp("m_cw"), Lp("m_cb"), Lp("m_dtb"), Lp("m_alog"), C("selh"), C("selg"),
                        gq, gk, gg, gv)
            phase_GLA(P, nseq, L, gq, gk, gg, gv, go, C("masks"))
            phase_MApost(P, T, go, gv, ptm, Lp("m_dsk"), Lp("m_nw"), mixT)
            phase_HGpre(P, nseq, l, pT, ptm, I["g_lbraw"], gq, gk, gg, gv)
            phase_GLA(P, nseq, L, gq, gk, gg, gv, go, C("masks"))
            phase_HGpost(P, T, go, ptm, Lp("hg_nw"), mixT)
        phase_O(P, nseq, mixT, w_out[l], X[l], X[l + 1], hn2, LG, Lp("wr"), Lp("nw2"))
        if do_moe:
            phase_R(P, nseq, LG, C("blk"), C("offs"), GT, IT)
            phase_E(P, nseq, wg[l], wu[l], wd[l], Lp("nw2"), hn2, GT, IT, X[l + 1], experts=experts)
    phase_F(P, T, X[depth], I["g_fw"], out)
    nc = P.finish()
    return P, nc, list(I.keys())


def host_inputs(inp, depth=DEPTH):
    m = {}
    for k, v in host_consts().items():
        m["c_" + k] = v
    for k, v in global_params(inp).items():
        m["g_" + k] = v
    for l in range(depth):
        for k, v in layer_params(inp, l).items():
            m["l%d_%s" % (l, k)] = v
    return m


N_CORES = 8
SEQ_PER_CORE = 4
_PROG = {}


def kernel(**inputs):
    inp = {k: np.asarray(v) for k, v in inputs.items()}
    if "p" not in _PROG:
        _PROG["p"] = build_program(SEQ_PER_CORE)
    P, nc, names = _PROG["p"]
    rep = host_inputs(inp)
    for k in ("w_in", "w_out", "moe_w_gate", "moe_w_up", "moe_w_down"):
        rep[k] = np.ascontiguousarray(inp[k], dtype=np.float32)
    x = np.ascontiguousarray(inp["x"], dtype=np.float32)
    in_maps = []
    for c in range(N_CORES):
        m = dict(rep)
        m["x"] = x[c * SEQ_PER_CORE:(c + 1) * SEQ_PER_CORE].reshape(SEQ_PER_CORE * L, D)
        in_maps.append({k: m[k] for k in names})
    res = run_bass_kernel_spmd(nc, in_maps, core_ids=list(range(N_CORES)))
    out = np.concatenate([np.asarray(r["out"]).reshape(SEQ_PER_CORE, L, D) for r in res.results], axis=0)
    return np.ascontiguousarray(out.astype(np.float32))
```

```python
import numpy as np
import ml_dtypes
from contextlib import ExitStack
import concourse.bass as bass
import concourse.mybir as mybir
from concourse.bass_utils import run_bass_kernel_spmd

F32 = mybir.dt.float32
BF16 = mybir.dt.bfloat16
I32 = mybir.dt.int32
U32 = mybir.dt.uint32
AF = mybir.ActivationFunctionType
ALU = mybir.AluOpType
AX = mybir.AxisListType


class Ref:
    __slots__ = ("ap", "tile", "key")

    def __init__(self, ap, tile, key):
        self.ap, self.tile, self.key = ap, tile, key

    def __getitem__(self, idx):
        return Ref(self.ap[idx], self.tile, self.key)

    def rr(self, pat, **kw):
        return Ref(self.ap.rearrange(pat, **kw), self.tile, self.key)

    def bc(self, shape):
        return Ref(self.ap.to_broadcast(list(shape)), self.tile, self.key)

    def bcast(self, shape):
        return Ref(self.ap.broadcast_to(list(shape)), self.tile, self.key)

    def pbc(self, n):
        return Ref(self.ap.partition_broadcast(n), self.tile, self.key)

    def bitcast(self, dt):
        return Ref(self.ap.bitcast(dt), self.tile, self.key)

    def unsq(self, ax):
        return Ref(self.ap.unsqueeze(ax), self.tile, self.key)

    def with_key(self, key):
        return Ref(self.ap, self.tile, key)


class _Keyed:
    def __init__(self, tile, key):
        self.tile, self.key = tile, key

    def __getitem__(self, idx):
        return Ref(self.tile._ap()[idx], self.tile, self.key)


class Tile:
    def __init__(self, handle, name, space):
        self.h, self.name, self.space = handle, name, space
        self.state = {}

    def _ap(self):
        return self.h.ap() if self.space == "dram" else self.h

    def __getitem__(self, idx):
        return Ref(self._ap()[idx], self, None)

    def k(self, key):
        return _Keyed(self, key)

    @property
    def all(self):
        return self[:]


class Instr:
    __slots__ = ("i", "eng", "fn", "deps", "dma", "signal", "sigidx", "dsem", "dval")

    def __init__(self, i, eng, fn, deps, dma):
        self.i, self.eng, self.fn, self.deps, self.dma = i, eng, fn, deps, dma
        self.signal = False
        self.sigidx = 0
        self.dsem = None
        self.dval = 0


ENGS = ("pe", "act", "dve", "pool", "sp")
DMAQ = ("sp", "act", "pool")
NDSEM = 8


class Prog:
    def __init__(self):
        self.nc = bass.Bass("TRN2", target_bir_lowering=False)
        self.instrs = []
        self.stack = ExitStack()
        self.pstack = None
        self.phase_start = 0
        nc = self.nc
        st = self.stack
        self.engsem = {e: st.enter_context(nc.semaphore("sem_" + e)) for e in ENGS}
        self.dsems = {e: [st.enter_context(nc.semaphore("dsem_%s_%d" % (e, j))) for j in range(NDSEM)]
                      for e in DMAQ}
        self.sigcount = {e: 0 for e in ENGS}
        self.dcount = {e: 0 for e in DMAQ}
        self.seen = {e: {} for e in ENGS}
        self._uid = 0

    def dram(self, name, shape, dtype, kind="Internal"):
        h = self.nc.dram_tensor(name, list(shape), dtype, kind=kind)
        return Tile(h, name, "dram")

    def _st(self):
        return self.pstack if self.pstack is not None else self.stack

    def sb(self, name, shape, dtype):
        self._uid += 1
        nm = "%s_%d" % (name, self._uid)
        h = self._st().enter_context(self.nc.sbuf_tensor(nm, list(shape), dtype))
        return Tile(h, nm, "sb")

    def ps(self, name, shape, dtype=F32):
        self._uid += 1
        nm = "%s_%d" % (name, self._uid)
        h = self._st().enter_context(self.nc.psum_tensor(nm, list(shape), dtype))
        return Tile(h, nm, "ps")

    def begin(self, name):
        assert self.pstack is None
        self.pstack = ExitStack()
        self.phase_start = len(self.instrs)
        self.phase_name = name

    def end(self):
        self._emit_phase()
        self.pstack.close()
        self.pstack = None

    @staticmethod
    def _overlap(state, key):
        if key is None:
            return list(state.keys())
        return [k for k in state.keys() if k is None or k == key]

    def _rec(self, eng, fn, reads, writes, dma=False):
        i = len(self.instrs)
        ps0 = self.phase_start
        deps = set()
        for r in reads:
            st = r.tile.state
            for k in self._overlap(st, r.key):
                w = st[k][0]
                if w is not None and w >= ps0:
                    deps.add(w)
        for w_ in writes:
            st = w_.tile.state
            for k in self._overlap(st, w_.key):
                e = st[k]
                if e[0] is not None and e[0] >= ps0:
                    deps.add(e[0])
                for rr_ in e[1]:
                    if rr_ >= ps0:
                        deps.add(rr_)
        for r in reads:
            st = r.tile.state
            e = st.get(r.key)
            if e is None:
                e = st[r.key] = [None, []]
            e[1] = [q for q in e[1] if q >= ps0]
            e[1].append(i)
        for w_ in writes:
            st = w_.tile.state
            if w_.key is None:
                st.clear()
                st[None] = [i, []]
            else:
                st[w_.key] = [i, []]
        deps.discard(i)
        ins = Instr(i, eng, fn, deps, dma)
        self.instrs.append(ins)
        return ins

    @staticmethod
    def _u(x):
        return x.ap if isinstance(x, Ref) else x

    @staticmethod
    def _refs(*xs):
        return [x for x in xs if isinstance(x, Ref)]

    def mm(self, out, lhsT, rhs, start=True, stop=True):
        u = self._u
        return self._rec("pe", lambda e: e.matmul(u(out), u(lhsT), u(rhs), start=start, stop=stop),
                         self._refs(lhsT, rhs), self._refs(out))

    def transpose(self, out, in_, ident):
        u = self._u
        return self._rec("pe", lambda e: e.transpose(u(out), u(in_), u(ident)),
                         self._refs(in_, ident), self._refs(out))

    def act(self, out, in_, func, bias=None, scale=1.0, accum=None, eng="act"):
        u = self._u
        kw = {}
        if bias is not None:
            kw["bias"] = u(bias)
        if accum is not None:
            kw["accum_out"] = u(accum)
        return self._rec(eng, lambda e: e.activation(u(out), u(in_), func, scale=u(scale), **kw),
                         self._refs(in_, bias, scale), self._refs(out, accum))

    def tt(self, eng, out, a, b, op):
        u = self._u
        return self._rec(eng, lambda e: e.tensor_tensor(u(out), u(a), u(b), op),
                         self._refs(a, b), self._refs(out))

    def ts(self, eng, out, a, s1, op0, s2=None, op1=None, accum=None):
        u = self._u
        kw = {}
        if accum is not None:
            kw["accum_out"] = u(accum)
        if op1 is None:
            return self._rec(eng, lambda e: e.tensor_scalar(u(out), u(a), u(s1), None, op0, **kw),
                             self._refs(a, s1), self._refs(out, accum))
        return self._rec(eng, lambda e: e.tensor_scalar(u(out), u(a), u(s1), u(s2), op0, op1, **kw),
                         self._refs(a, s1, s2), self._refs(out, accum))

    def stt(self, out, a, s, b, op0, op1, eng="dve"):
        u = self._u
        return self._rec(eng, lambda e: e.scalar_tensor_tensor(u(out), u(a), u(s), u(b), op0, op1),
                         self._refs(a, s, b), self._refs(out))

    def copy(self, eng, out, in_):
        u = self._u
        if eng == "act":
            return self._rec(eng, lambda e: e.copy(u(out), u(in_)), self._refs(in_), self._refs(out))
        return self._rec(eng, lambda e: e.tensor_copy(u(out), u(in_)), self._refs(in_), self._refs(out))

    def memset(self, eng, out, val):
        u = self._u
        return self._rec(eng, lambda e: e.memset(u(out), val), [], self._refs(out))

    def reduce(self, out, in_, op, axis=AX.X, eng="dve"):
        u = self._u
        return self._rec(eng, lambda e: e.tensor_reduce(u(out), u(in_), axis, op),
                         self._refs(in_), self._refs(out))

    def recip(self, out, in_):
        u = self._u
        return self._rec("dve", lambda e: e.reciprocal(u(out), u(in_)), self._refs(in_), self._refs(out))

    def scan(self, out, d0, d1, init, op0, op1):
        u = self._u
        return self._rec("dve", lambda e: e.tensor_tensor_scan(u(out), u(d0), u(d1), u(init), op0, op1),
                         self._refs(d0, d1, init), self._refs(out))

    def generic(self, eng, fn, reads, writes):
        return self._rec(eng, fn, reads, writes)

    def dma(self, q, out, in_, **kw):
        u = self._u
        return self._rec(q, lambda e: e.dma_start(out=u(out), in_=u(in_), **kw),
                         self._refs(in_), self._refs(out), dma=True)

    def dma_generic(self, q, fn, reads, writes):
        return self._rec(q, fn, reads, writes, dma=True)


    def _emit_phase(self):
        nc = self.nc
        instrs = self.instrs
        ps0 = self.phase_start
        cur = instrs[ps0:]
        per = {e: [] for e in ENGS}
        for ins in cur:
            per[ins.eng].append(ins)
        for ins in cur:
            for d in ins.deps:
                D = instrs[d]
                if D.dma:
                    continue
                if D.eng == "pe" and ins.eng == "pe":
                    continue
                D.signal = True
        for e in ENGS:
            for ins in per[e]:
                if ins.signal:
                    self.sigcount[e] += 1
                    ins.sigidx = self.sigcount[e]
        for e in DMAQ:
            for ins in per[e]:
                if ins.dma:
                    k = self.dcount[e]
                    ins.dsem = self.dsems[e][k % NDSEM]
                    ins.dval = 16 * (k // NDSEM + 1)
                    self.dcount[e] = k + 1
        engsem = self.engsem
        with nc.Block() as block:
            def run(ename, eng):
                seen = self.seen[ename]

                def wait(sem, val):
                    if seen.get(sem.name, 0) >= val:
                        return
                    eng.wait_ge(sem, val)
                    seen[sem.name] = val

                last = {}
                for ins in per[ename]:
                    for d in sorted(ins.deps):
                        D = instrs[d]
                        if D.dma:
                            wait(D.dsem, D.dval)
                        else:
                            if D.eng == "pe" and ename == "pe":
                                continue
                            wait(engsem[D.eng], D.sigidx)
                    if ins.dma and ins.dval > 16:
                        wait(ins.dsem, ins.dval - 16)
                    bi = ins.fn(eng)
                    if ins.dma:
                        bi.then_inc(ins.dsem, 16)
                        last[ins.dsem.name] = (ins.dsem, ins.dval)
                    elif ins.signal:
                        bi.then_inc(engsem[ename], 1)
                for (sem, val) in last.values():
                    wait(sem, val)

            block.tensor(lambda eng: run("pe", eng))
            block.scalar(lambda eng: run("act", eng))
            block.vector(lambda eng: run("dve", eng))
            block.gpsimd(lambda eng: run("pool", eng))
            block.sync(lambda eng: run("sp", eng))

    def finish(self):
        self.stack.close()
        return self.nc

import math

D = 1024
L = 2048
NT = 16
EPS = 1e-6
FM_SEGS = [(0, 768, 0), (1024, 512, 768), (1544, 512, 1280), (2312, 768, 1792), (1536, 8, 2560)]
NFM = 2568
TM_SEGS = [(768, 256, 0), (2056, 256, 256), (3080, 512, 512)]
NTM = 1024
R_HY, R_MX, R_AT, R_HG, R_DT = 0, 768, 1280, 1792, 2560
C_MZ, C_AV, C_HI, C_HGATE = 0, 256, 512, 768


class Rot:
    def __init__(self, tiles):
        self.t, self.i = tiles, 0

    def next(self):
        t = self.t[self.i % len(self.t)]
        self.i += 1
        return t


def make_ident(P, dtype=BF16):
    idf = P.sb("identf", [128, 128], F32)
    P.memset("pool", idf[:], 0.0)
    P.generic("pool", lambda e: e.affine_select(idf.h[:], idf.h[:], [[-1, 128]], ALU.not_equal, 1.0,
                                                base=0, channel_multiplier=1), [idf[:]], [idf[:]])
    if dtype == F32:
        return idf
    idb = P.sb("identb", [128, 128], BF16)
    P.copy("dve", idb[:], idf[:])
    return idb


def rms_rstd(P, ss, rstd, n):
    P.ts("dve", rstd[:], ss[:], 1.0 / n, ALU.mult, EPS, ALU.add)
    P.act(rstd[:], rstd[:], AF.Sqrt)
    P.recip(rstd[:], rstd[:])


def phase_P(P, T, Xin, w_in, nw, pT, ptm):
    P.begin("P")
    Wfm = P.sb("Wfm", [128, 8, NFM], BF16)
    Wtm = P.sb("Wtm", [128, 8, NTM], BF16)
    nwt = P.sb("nwt", [128, 8], F32)
    P.dma("sp", nwt[:], nw[:])
    stg = Rot([P.sb("stg", [128, 3592], F32) for _ in range(3)])
    for k in range(8):
        st = stg.next()
        P.dma("sp" if k % 2 == 0 else "act", st[:], w_in[k * 128:(k + 1) * 128, :])
        segs = [(Wfm, a) for a in FM_SEGS] + [(Wtm, a) for a in TM_SEGS]
        for si, (Wt, (s0, n, d0)) in enumerate(segs):
            eng = ("dve", "act", "pool")[(si + k) % 3]
            if eng == "act":
                P.act(Wt[:, k, d0:d0 + n], st[:, s0:s0 + n], AF.Copy, scale=nwt[:, k:k + 1])
            else:
                P.ts(eng, Wt[:, k, d0:d0 + n], st[:, s0:s0 + n], nwt[:, k:k + 1], ALU.mult)
    ident = make_ident(P)
    xb = Rot([P.sb("xb", [128, D], F32) for _ in range(3)])
    sq = P.sb("sq", [128, D], F32)
    ssb = Rot([P.sb("ss", [128, 1], F32) for _ in range(4)])
    rsb = Rot([P.sb("rs", [128, 1], F32) for _ in range(4)])
    hTb = Rot([P.sb("hT", [128, 8, 512], BF16) for _ in range(2)])
    psT = Rot([P.ps("psT", [128, 8, 128], BF16) for _ in range(2)])
    pstm = Rot([P.ps("pstm", [128, 512], F32) for _ in range(2)])
    psfm = Rot([P.ps("psfm", [128, 512], F32) for _ in range(3)])
    otm = Rot([P.sb("otm", [128, NTM], F32) for _ in range(2)])
    ofm = Rot([P.sb("ofm", [128, 512], F32) for _ in range(3)])
    nch = (NFM + 127) // 128
    hnb = Rot([P.sb("hn8", [128, D], BF16) for _ in range(8)])

    def norms(g):
        res = []
        for i4 in range(4):
            t0 = (g * 4 + i4) * 128
            xt = xb.next()
            P.dma("sp", xt[:], Xin[t0:t0 + 128, :])
            ss, rs, hn = ssb.next(), rsb.next(), hnb.next()
            P.act(sq[:], xt[:], AF.Square, accum=ss[:])
            rms_rstd(P, ss, rs, D)
            P.ts("dve", hn[:], xt[:], rs[:], ALU.mult)
            res.append(hn)
        return res

    ng = T // 512
    hns = norms(0)
    for g in range(ng):
        hT = hTb.next()
        cur_hn = hns
        for i4 in range(4):
            t0 = (g * 4 + i4) * 128
            hn = cur_hn[i4]
            pt = psT.next()
            for k in range(8):
                P.transpose(pt[:, k, :], hn[:, k * 128:(k + 1) * 128], ident[:])
            P.copy("act", hT[:, :, i4 * 128:(i4 + 1) * 128], pt[:])
        if g + 1 < ng:
            hns = norms(g + 1)
        for i4 in range(4):
            t0 = (g * 4 + i4) * 128
            o = otm.next()
            for half in range(2):
                pm = pstm.next()
                for k in range(8):
                    P.mm(pm[:], hT[:, k, i4 * 128:(i4 + 1) * 128], Wtm[:, k, half * 512:(half + 1) * 512],
                         start=(k == 0), stop=(k == 7))
                P.copy("dve" if half == 0 else "act", o[:, half * 512:(half + 1) * 512], pm[:])
            P.dma("pool", ptm[t0:t0 + 128, :], o[:])
        for c in range(nch):
            n = min(128, NFM - c * 128)
            pf = psfm.next()
            for k in range(8):
                P.mm(pf[0:n, :], Wfm[:, k, c * 128:c * 128 + n], hT[:, k, :], start=(k == 0), stop=(k == 7))
            of = ofm.next()
            P.copy("dve" if c % 2 == 0 else "act", of[0:n, :], pf[0:n, :])
            P.dma("pool", pT[c * 128:c * 128 + n, g * 512:(g + 1) * 512], of[0:n, :])
    P.end()


def gla_masks():
    s = np.arange(128)[:, None]
    t = np.arange(128)[None, :]
    same32 = (s // 32) == (t // 32)
    mCf = same32 & (s <= t)
    mCb = same32 & (s >= t)
    mABf = ((s < 64) & (t >= 64)) | ((s < 32) & (t >= 32) & (t < 64)) | ((s >= 64) & (s < 96) & (t >= 96))
    mABb = mABf.T
    return np.stack([np.concatenate([mCf, mABf], 1), np.concatenate([mCb, mABb], 1)]).astype(np.uint32)


def bank(P, name):
    return P.ps(name, [128, 512], F32)


def cpred(P, out, mask, data):
    return P.generic("dve", lambda e: e.copy_predicated(out.ap, mask.ap, data.ap), [mask, data], [out])


def phase_GLA(P, nseq, Ls, gq, gk, gg, gv, go, masks_d):
    P.begin("GLA")
    nt = Ls // 128
    ident = make_ident(P)
    msk = P.sb("msk", [128, 2, 256], U32)
    P.dma("sp", msk[:], masks_d[:].rr("m p t -> p m t"))
    qb = Rot([P.sb("q", [128, Ls], BF16) for _ in range(1)])
    kb = Rot([P.sb("k", [128, Ls], BF16) for _ in range(1)])
    gb = Rot([P.sb("g", [128, Ls], F32) for _ in range(1)])
    Fb = Rot([P.sb("F", [128, Ls + 1], F32) for _ in range(1)])
    vb = Rot([P.sb("v", [128, nt, 128], BF16) for _ in range(2)])
    Dt = Rot([P.sb("Dt", [128, Ls], F32) for _ in range(3)])
    Et = Rot([P.sb("Et", [128, Ls], BF16) for _ in range(4)])
    names = ["qT", "kT", "qA", "kA", "qB", "kB", "qC", "kC"]
    der = {n: Rot([P.sb(n, [128, Ls], BF16) for _ in range(2)]) for n in names}
    ktm = Rot([P.sb("ktm", [128, nt, 128], BF16) for _ in range(2)])
    aT = Rot([P.sb("aT", [128, nt], F32) for _ in range(2)])
    oacc = P.sb("oacc", [128, nt, 256], F32)
    Gt = {d: Rot([P.sb("Gt%d" % d, [128, 256], BF16) for _ in range(4)]) for d in range(2)}
    for d in range(2):
        for t_ in Gt[d].t:
            P.memset("pool", t_[:], 0.0)
    Sin32b = Rot([P.sb("Sin32", [128, nt, 128], F32) for _ in range(2)])
    Sinbfb = Rot([P.sb("Sinbf", [128, nt, 128], BF16) for _ in range(2)])
    psC = Rot([bank(P, "psC") for _ in range(4)])
    psO = Rot([bank(P, "psO") for _ in range(2)])
    psU = bank(P, "psU")
    psK = P.ps("psK", [128, 8, 128], BF16)
    z0 = Gt[0].t[0]
    for pc_ in psC.t:
        P.mm(pc_[:, 0:256], z0[:, 0:128], z0[:, 0:256])
    mul_eng = {"qC": "dve", "kC": "pool", "qB": "dve", "kB": "pool", "qA": "dve", "kA": "pool", "qT": "dve", "kT": "dve"}

    def make_prep(s, d, c):
        c0 = s * Ls
        q, k, g, F, v = qb.next(), kb.next(), gb.next(), Fb.next(), vb.next()
        a, km = aT.next(), ktm.next()
        Sin32, Sinbf = Sin32b.next(), Sinbfb.next()
        dv = {}
        ctx = dict(v=v, a=a, km=km, dv=dv, Sinbf=Sinbf)
        th = []
        big_ids = set()
        ctx["big"] = big_ids
        rows = slice(c * 128, (c + 1) * 128)
        th.append(lambda: P.dma("sp", q[:], gq[rows, c0:c0 + Ls]))
        th.append(lambda: P.dma("sp", k[:], gk[d, rows, c0:c0 + Ls]))
        th.append(lambda: P.dma("sp", g[:], gg[d, rows, c0:c0 + Ls]))
        th.append(lambda: P.dma("sp", v[:], gv[c0:c0 + Ls, rows].rr("(n p) c -> p n c", p=128)))
        th.append(lambda: P.memset("pool", F[:, 0:1], 0.0))
        th.append(lambda: P.scan(F[:, 1:Ls + 1], g[:], g[:], 0.0, ALU.add, ALU.bypass))
        Fsrc = F[:, 1:Ls + 1] if d == 0 else F[:, 0:Ls]
        sq, sk = (1.0, -1.0) if d == 0 else (-1.0, 1.0)

        def refs(blk, refoff):
            nb = Ls // blk
            return F[:, refoff:refoff + (nb - 1) * blk + 1:blk].unsq(2).bcast([128, nb, blk])

        order = list(range(nt)) if d == 0 else list(range(nt - 1, -1, -1))

        def mk_sub(blk, refoff):
            Dm = Dt.next()
            return Dm, (lambda: P.tt("pool", Dm[:].rr("p (n b) -> p n b", b=blk), Fsrc.rr("p (n b) -> p n b", b=blk),
                                     refs(blk, refoff), ALU.subtract))

        def mk_exp_mul(nm, base, Dm, sgn):
            E = Et.next()
            o = der[nm].next()
            dv[nm] = o
            H = Ls // 2

            def ex():
                P.act(E[:, 0:H], Dm[:, 0:H], AF.Exp, scale=sgn)
                P.act(E[:, H:Ls], Dm[:, H:Ls], AF.Exp, scale=sgn)

            def mu():
                P.tt(mul_eng[nm], o[:, 0:H], base[:, 0:H], E[:, 0:H], ALU.mult)
                P.tt(mul_eng[nm], o[:, H:Ls], base[:, H:Ls], E[:, H:Ls], ALU.mult)
            return ex, mu

        D1, S1 = mk_sub(128, 0 if d == 0 else 128)
        D2, S2 = mk_sub(128, 128 if d == 0 else 0)
        D3 = Dt.next()
        S3 = lambda: P.tt("pool", D3[:, 0:nt], F[:, 128:Ls + 1:128], F[:, 0:Ls:128], ALU.subtract)
        Ea = lambda: P.act(a[:], D3[:, 0:nt], AF.Exp)
        E1, M1 = mk_exp_mul("qT", q, D1, sq)
        E2, M2 = mk_exp_mul("kT", k, D2, sk)
        D4, S4 = mk_sub(32, 16)
        E4q, M4q = mk_exp_mul("qC", q, D4, sq)
        E4k, M4k = mk_exp_mul("kC", k, D4, sk)
        D5, S5 = mk_sub(64, 32)
        E5q, M5q = mk_exp_mul("qB", q, D5, sq)
        E5k, M5k = mk_exp_mul("kB", k, D5, sk)
        D6, S6 = mk_sub(128, 64)
        E6q, M6q = mk_exp_mul("qA", q, D6, sq)
        E6k, M6k = mk_exp_mul("kA", k, D6, sk)
        tr = []
        for T4 in range(0, nt, 8):
            n8 = min(8, nt - T4)
            for j in range(n8):
                tr.append((lambda T4=T4, j=j: lambda: P.transpose(psK[:, j, :], dv["kT"][:, (T4 + j) * 128:(T4 + j + 1) * 128], ident[:]))())
            tr.append((lambda T4=T4, n8=n8: lambda: P.copy("act", km[:, T4:T4 + n8, :], psK[:, 0:n8, :]))())
        st = [lambda: P.memset("pool", Sin32[:, order[0], :], 0.0)]
        for i in range(0, nt, 4):
            grp = order[i:i + 4]
            for j, T_ in enumerate(grp):
                st.append((lambda j=j, T_=T_: lambda: P.mm(psU[:, j * 128:(j + 1) * 128], km[:, T_, :], v[:, T_, :]))())
            for j, T_ in enumerate(grp):
                idx = i + j
                if idx + 1 < nt:
                    Tn = order[idx + 1]
                    st.append((lambda j=j, T_=T_, Tn=Tn: lambda: P.stt(Sin32[:, Tn, :], Sin32[:, T_, :], a[:, T_:T_ + 1],
                                                                     psU[:, j * 128:(j + 1) * 128], ALU.mult, ALU.add))())
        st.append(lambda: P.copy("act", Sinbf[:], Sin32[:]))
        for f_ in (S1, S2, S4, S5, S6, E1, E2, E4q, E4k, E5q, E5k, E6q, E6k, M1, M2, M4q, M4k, M5q, M5k, M6q, M6k):
            big_ids.add(id(f_))
        th += [S2, S1, S3, E2, E1, S4, M2, Ea, S5, M1, E4q, E4k]
        th += tr
        th += [S6, M4q, M4k, E5q, E5k]
        th += st
        th += [M5q, M5k, E6q, E6k, M6q, M6k]
        return ctx, th

    units = [(s, d, c) for s in range(nseq) for d in range(2) for c in range(2)]
    cur = make_prep(*units[0])
    for t_ in cur[1]:
        t_()
    for ui, (s, d, c) in enumerate(units):
        ctx = cur[0]
        v, a, km, dv, Sinbf = ctx["v"], ctx["a"], ctx["km"], ctx["dv"], ctx["Sinbf"]
        nxt_th = []
        if ui + 1 < len(units):
            cur = make_prep(*units[ui + 1])
            nxt_th = list(cur[1])
        wts = [6.0 if id(f_) in cur[0]["big"] else 1.0 for f_ in nxt_th] if nxt_th else []
        tot_w = sum(wts)
        done_w = 0.0
        c0 = s * Ls
        order = list(range(nt)) if d == 0 else list(range(nt - 1, -1, -1))
        m2 = msk[:, d, :]
        kA, qA, kB, qB, kC, qC = dv["kA"], dv["qA"], dv["kB"], dv["qB"], dv["kC"], dv["qC"]

        def scores(T_):
            t0 = T_ * 128
            res = []
            for h in range(2):
                hr = slice(h * 64, (h + 1) * 64)
                pc = psC.next()
                P.mm(pc[:, 0:128], kC[hr, t0:t0 + 128], qC[hr, t0:t0 + 128])
                if d == 0:
                    P.mm(pc[0:64, 192:256], kA[hr, t0:t0 + 64], qA[hr, t0 + 64:t0 + 128])
                    P.mm(pc[0:32, 160:192], kB[hr, t0:t0 + 32], qB[hr, t0 + 32:t0 + 64])
                    P.mm(pc[64:96, 224:256], kB[hr, t0 + 64:t0 + 96], qB[hr, t0 + 96:t0 + 128])
                else:
                    P.mm(pc[64:128, 128:192], kA[hr, t0 + 64:t0 + 128], qA[hr, t0:t0 + 64])
                    P.mm(pc[0:64, 128:160], kB[hr, t0:t0 + 64], qB[hr, t0:t0 + 32])
                    P.mm(pc[64:128, 192:224], kB[hr, t0 + 64:t0 + 128], qB[hr, t0 + 64:t0 + 96])
                G = Gt[d].next()
                cpred(P, G[:], m2, pc[:, 0:256])
                res.append(G)
            return res

        nxt = scores(order[0])
        for oi, T_ in enumerate(order):
            t0 = T_ * 128
            Gs = nxt
            if oi + 1 < nt:
                nxt = scores(order[oi + 1])
            po = psO.next()
            for h in range(2):
                hr = slice(h * 64, (h + 1) * 64)
                oc = slice(h * 64, (h + 1) * 64)
                P.mm(po[:, oc], Gs[h][:, 0:128], v[:, T_, oc], start=True, stop=False)
                P.mm(po[:, oc], Gs[h][:, 128:256], v[:, T_, oc], start=False, stop=False)
                P.mm(po[:, oc], dv["qT"][hr, t0:t0 + 128], Sinbf[hr, T_, oc], start=False, stop=True)
            oa = oacc[:, T_, c * 128:(c + 1) * 128]
            if d == 0:
                P.copy("act", oa, po[:, 0:128])
            else:
                P.tt("dve", oa, oa, po[:, 0:128], ALU.add)
            while nxt_th and done_w < tot_w * (oi + 1) / nt:
                done_w += wts.pop(0)
                nxt_th.pop(0)()
        while nxt_th:
            nxt_th.pop(0)()
        if d == 1 and c == 1:
            for T_ in range(nt):
                P.dma("act", go[c0 + T_ * 128:c0 + (T_ + 1) * 128, :], oacc[:, T_, :])
    P.end()


def phase_MApre(P, nseq, pT, cw_d, cb_d, dtb_d, alog_d, selh_d, selg_d, gq, gk, gg, gv):
    P.begin("MApre")
    ident = make_ident(P)
    cw = P.sb("cw", [128, 4, 5], F32)
    cb = P.sb("cb", [128, 4], F32)
    dtb = P.sb("dtb", [8, 1], F32)
    aneg = P.sb("aneg", [8, 1], F32)
    selh = P.sb("selh", [8, 4, 128], F32)
    selg = P.sb("selg", [128, 2, 128], BF16)
    P.dma("sp", cw[:], cw_d[:])
    P.dma("sp", cb[:], cb_d[:])
    P.dma("sp", dtb[:], dtb_d[:])
    P.dma("sp", aneg[:], alog_d[:])
    P.dma("sp", selh[:], selh_d[:].rr("j d c m -> j (d c) m"))
    P.dma("sp", selg[:], selg_d[:])
    P.act(aneg[:], aneg[:], AF.Exp)
    P.ts("dve", aneg[:], aneg[:], -1.0, ALU.mult)
    Pp = Rot([P.sb("Pp", [128, L + 4], F32) for _ in range(2)])
    for t_ in Pp.t:
        P.memset("pool", t_[:, 0:2], 0.0)
        P.memset("pool", t_[:, L + 2:L + 4], 0.0)
    acc = Rot([P.sb("acc", [128, L], F32) for _ in range(2)])
    xbc = [P.sb("xbc%d" % c, [128, L], BF16) for c in range(4)]
    dtr = P.sb("dtr", [8, L], F32)
    dt = P.sb("dt", [8, L], F32)
    dta = P.sb("dta", [8, L], F32)
    vt = P.sb("vt", [128, NT, 256], BF16)
    rep = Rot([P.sb("rep", [128, 512], F32) for _ in range(2)])
    ob = Rot([P.sb("ob", [128, L], BF16) for _ in range(2)])
    og = Rot([P.sb("og", [128, L], F32) for _ in range(2)])
    psA = Rot([bank(P, "psA") for _ in range(2)])
    psB = Rot([bank(P, "psB") for _ in range(2)])
    psT = Rot([P.ps("psT", [128, 8, 128], BF16) for _ in range(2)])
    for s in range(nseq):
        c0 = s * L
        for c in range(4):
            pp = Pp.next()
            P.dma("sp", pp[:, 2:L + 2], pT[R_MX + c * 128:R_MX + (c + 1) * 128, c0:c0 + L])
            a = acc.next()
            P.ts("dve", a[:], pp[:, 0:L], cw[:, c, 0:1], ALU.mult)
            for j in range(1, 5):
                P.stt(a[:], pp[:, j:j + L], cw[:, c, j:j + 1], a[:], ALU.mult, ALU.add)
            P.act(xbc[c][:], a[:], AF.Silu, bias=cb[:, c:c + 1])
        P.dma("sp", dtr[:], pT[R_DT:R_DT + 8, c0:c0 + L])
        P.act(dt[:], dtr[:], AF.Exp, bias=dtb[:])
        P.act(dt[:], dt[:], AF.Ln, bias=1.0)
        P.ts("dve", dta[:], dt[:], aneg[:], ALU.mult)
        for T8 in range(0, NT, 4):
            pt = psT.next()
            for j in range(4):
                for c in range(2):
                    P.transpose(pt[:, j * 2 + c, :], xbc[c][:, (T8 + j) * 128:(T8 + j + 1) * 128], ident[:])
            P.copy("act", vt[:, T8:T8 + 4, :].rr("p n (c m) -> p (n c) m", c=2), pt[:])
        P.dma("act", gv[c0:c0 + L, :].rr("(n p) c -> p n c", p=128), vt[:])
        for c in range(2):
            oq = ob.next()
            for g4 in range(4):
                cs = slice(g4 * 512, (g4 + 1) * 512)
                pa = psA.next()
                P.mm(pa[:], selg[:, c, :], xbc[3][:, cs])
                P.copy("act", oq[:, cs], pa[:])
            P.dma("act", gq[c * 128:(c + 1) * 128, c0:c0 + L], oq[:])
            for d in range(2):
                ok_, og_ = ob.next(), og.next()
                for g4 in range(4):
                    cs = slice(g4 * 512, (g4 + 1) * 512)
                    pa = psA.next()
                    P.mm(pa[:], selg[:, c, :], xbc[2][:, cs])
                    r = rep.next()
                    P.copy("act", r[:], pa[:])
                    pb = psB.next()
                    P.mm(pb[:], selh[:, d * 2 + c, :], dt[:, cs])
                    P.tt("dve", ok_[:, cs], r[:], pb[:], ALU.mult)
                    pb2 = psB.next()
                    P.mm(pb2[:], selh[:, d * 2 + c, :], dta[:, cs])
                    P.copy("act", og_[:, cs], pb2[:])
                P.dma("act", gk[d, c * 128:(c + 1) * 128, c0:c0 + L], ok_[:])
                P.dma("act", gg[d, c * 128:(c + 1) * 128, c0:c0 + L], og_[:])
    P.end()


def tm_to_mixT(P, ident, psT, stage, yb, i4):
    pt = psT.next()
    for c in range(2):
        P.transpose(pt[:, c, :], yb[:, c * 128:(c + 1) * 128], ident[:])
    P.copy("act", stage[:, :, i4 * 128:(i4 + 1) * 128], pt[:, 0:2, :])


def phase_MApost(P, T, go, gv, ptm, dsk_d, nw_d, mixT):
    P.begin("MApost")
    ident = make_ident(P)
    dsk = P.sb("dsk", [128, 256], F32)
    nwt = P.sb("nwt", [128, 256], F32)
    P.dma("sp", dsk[:], dsk_d[:])
    P.dma("sp", nwt[:], nw_d[:])
    ob = Rot([P.sb("o", [128, 256], F32) for _ in range(4)])
    xb = Rot([P.sb("xs", [128, 256], BF16) for _ in range(4)])
    zb = Rot([P.sb("z", [128, 256], F32) for _ in range(4)])
    yb = Rot([P.sb("y", [128, 256], F32) for _ in range(4)])
    sq = P.sb("sq", [128, 256], F32)
    ssb = Rot([P.sb("ss", [128, 1], F32) for _ in range(4)])
    rsb = Rot([P.sb("rs", [128, 1], F32) for _ in range(4)])
    ybf = Rot([P.sb("ybf", [128, 256], BF16) for _ in range(4)])
    stg = Rot([P.sb("stg", [128, 2, 512], BF16) for _ in range(2)])
    psT = Rot([P.ps("psT", [128, 8, 128], BF16) for _ in range(2)])
    for g in range(T // 512):
        st = stg.next()
        for i4 in range(4):
            t0 = (g * 4 + i4) * 128
            o, xs, z, y = ob.next(), xb.next(), zb.next(), yb.next()
            P.dma("sp", o[:], go[t0:t0 + 128, :])
            P.dma("sp", xs[:], gv[t0:t0 + 128, :])
            P.dma("sp", z[:], ptm[t0:t0 + 128, C_MZ:C_MZ + 256])
            P.tt("dve", y[:], xs[:], dsk[:], ALU.mult)
            P.tt("dve", y[:], y[:], o[:], ALU.add)
            P.act(z[:], z[:], AF.Silu)
            P.tt("dve", y[:], y[:], z[:], ALU.mult)
            ss, rs = ssb.next(), rsb.next()
            P.act(sq[:], y[:], AF.Square, accum=ss[:])
            rms_rstd(P, ss, rs, 256)
            yo = ybf.next()
            P.stt(yo[:], y[:], rs[:], nwt[:], ALU.mult, ALU.mult)
            tm_to_mixT(P, ident, psT, st, yo, i4)
        P.dma("act", mixT[256:512, g * 512:(g + 1) * 512].rr("(c p) t -> p c t", p=128), st[:])
    P.end()


def phase_HGpre(P, nseq, layer, pT, ptm, lbraw_d, gq, gk, gg, gv):
    P.begin("HGpre")
    lbr = P.sb("lbr", [128, 2, 4], F32)
    P.dma("sp", lbr[:], lbraw_d[:].rr("p l d c -> p l (d c)"))
    lb = P.sb("lb", [128, 4], F32)
    oml = P.sb("oml", [128, 4], F32)
    s0 = P.sb("s0", [128, 4], F32)
    P.tt("dve", s0[:], lbr[:, 0, :], lbr[:, 1, :], ALU.subtract)
    P.act(s0[:], s0[:], AF.Sigmoid)
    if layer == 0:
        P.tt("dve", lb[:], s0[:], s0[:], ALU.subtract)
    else:
        s1 = P.sb("s1", [128, 4], F32)
        P.tt("dve", s1[:], lbr[:, 1, :], lbr[:, 0, :], ALU.subtract)
        P.act(s1[:], s1[:], AF.Sigmoid)
        P.tt("dve", lb[:], s0[:], s1[:], ALU.add)
        P.tt("dve", lb[:], lb[:], s0[:], ALU.subtract)
    P.ts("dve", oml[:], lb[:], -1.0, ALU.mult, 1.0, ALU.add)
    inb = Rot([P.sb("in", [128, L], F32) for _ in range(3)])
    sg = Rot([P.sb("sg", [128, L], F32) for _ in range(2)])
    ob = Rot([P.sb("ob", [128, L], BF16) for _ in range(3)])
    og = Rot([P.sb("og", [128, L], F32) for _ in range(2)])
    vi = Rot([P.sb("vi", [128, NT, 256], F32) for _ in range(1)])
    vo = Rot([P.sb("vo", [128, NT, 256], BF16) for _ in range(1)])
    for s in range(nseq):
        c0 = s * L
        for c in range(2):
            x = inb.next()
            P.dma("sp", x[:], pT[R_HG + c * 128:R_HG + (c + 1) * 128, c0:c0 + L])
            o = ob.next()
            P.act(o[:], x[:], AF.Silu)
            P.dma("pool", gq[c * 128:(c + 1) * 128, c0:c0 + L], o[:])
            for d in range(2):
                x = inb.next()
                r0 = R_HG + 256 + d * 256 + c * 128
                P.dma("sp", x[:], pT[r0:r0 + 128, c0:c0 + L])
                j = d * 2 + c
                sgm = sg.next()
                P.act(sgm[:], x[:], AF.Sigmoid)
                P.ts("dve", sgm[:], sgm[:], oml[:, j:j + 1], ALU.mult, lb[:, j:j + 1], ALU.add)
                g_ = og.next()
                P.act(g_[:], sgm[:], AF.Ln)
                P.dma("pool", gg[d, c * 128:(c + 1) * 128, c0:c0 + L], g_[:])
                sk = sg.next()
                P.act(sk[:], x[:], AF.Sigmoid, scale=-1.0)
                k_ = ob.next()
                P.ts("dve", k_[:], sk[:], oml[:, j:j + 1], ALU.mult)
                P.dma("pool", gk[d, c * 128:(c + 1) * 128, c0:c0 + L], k_[:])
        v_in, v_out = vi.next(), vo.next()
        P.dma("sp", v_in[:], ptm[c0:c0 + L, C_HI:C_HI + 256].rr("(n p) c -> p n c", p=128))
        P.copy("pool", v_out[:], v_in[:])
        P.dma("pool", gv[c0:c0 + L, :].rr("(n p) c -> p n c", p=128), v_out[:])
    P.end()


def phase_HGpost(P, T, go, ptm, nw_d, mixT):
    P.begin("HGpost")
    ident = make_ident(P)
    nwt = P.sb("nwt", [128, 256], F32)
    P.dma("sp", nwt[:], nw_d[:])
    ob = Rot([P.sb("o", [128, 256], F32) for _ in range(4)])
    zb = Rot([P.sb("z", [128, 256], F32) for _ in range(4)])
    sq = P.sb("sq", [128, 256], F32)
    ssb = Rot([P.sb("ss", [128, 4], F32) for _ in range(4)])
    ybf = Rot([P.sb("ybf", [128, 256], BF16) for _ in range(4)])
    stg = Rot([P.sb("stg", [128, 2, 512], BF16) for _ in range(2)])
    psT = Rot([P.ps("psT", [128, 8, 128], BF16) for _ in range(2)])
    for g in range(T // 512):
        st = stg.next()
        for i4 in range(4):
            t0 = (g * 4 + i4) * 128
            o, z = ob.next(), zb.next()
            P.dma("sp", o[:], go[t0:t0 + 128, :])
            P.dma("sp", z[:], ptm[t0:t0 + 128, C_HGATE:C_HGATE + 256])
            ss = ssb.next()
            P.tt("pool", sq[:], o[:], o[:], ALU.mult)
            P.reduce(ss[:], sq[:].rr("p (h e) -> p h e", e=64), ALU.add)
            P.ts("dve", ss[:], ss[:], 1.0 / 64, ALU.mult, EPS, ALU.add)
            P.act(ss[:], ss[:], AF.Sqrt)
            P.recip(ss[:], ss[:])
            P.tt("dve", o[:].rr("p (h e) -> p h e", e=64), o[:].rr("p (h e) -> p h e", e=64),
                 ss[:].unsq(2).bcast([128, 4, 64]), ALU.mult)
            P.act(z[:], z[:], AF.Silu)
            P.tt("dve", o[:], o[:], nwt[:], ALU.mult)
            yo = ybf.next()
            P.tt("dve", yo[:], o[:], z[:], ALU.mult)
            tm_to_mixT(P, ident, psT, st, yo, i4)
        P.dma("act", mixT[768:1024, g * 512:(g + 1) * 512].rr("(c p) t -> p c t", p=128), st[:])
    P.end()


def phase_ATE(P, rbrep_d, onehot_d, multrep_d, Mtab):
    P.begin("ATE")
    rb = P.sb("rb", [32, 4, 128], F32)
    oh = P.sb("oh", [32, 4096], F32)
    mu = P.sb("mu", [128, 4096], F32)
    P.dma("sp", rb[:], rbrep_d[:].rr("h b m -> b h m"))
    P.dma("sp", oh[:], onehot_d[:])
    P.dma("sp", mu[:], multrep_d[:])
    row = Rot([P.sb("row", [128, 4096], BF16) for _ in range(2)])
    eb = Rot([P.sb("eb", [128, 512], F32) for _ in range(2)])
    ps = Rot([bank(P, "ps") for _ in range(2)])
    for h in range(4):
        r = row.next()
        for ch in range(8):
            cs = slice(ch * 512, (ch + 1) * 512)
            p = ps.next()
            P.mm(p[:], rb[:, h, :], oh[:, cs])
            e = eb.next()
            P.act(e[:], p[:], AF.Exp)
            P.tt("dve", r[:, cs], e[:], mu[:, cs], ALU.mult)
        P.dma("sp", Mtab[h], r[:])
    P.end()


def phase_AT(P, nseq, pT, ptm, Mtab, selat_d, mixT):
    P.begin("AT")
    SW = 31 * 128
    strips = P.sb("strips", [128, 4, SW], BF16)
    for h in range(4):
        src = bass.AP(Mtab.h, h * 128 * 4096 + 127, [[4095, 128], [1, SW]])
        P.dma("sp", strips[:, h, :], Ref(src, Mtab, None))
    selat = P.sb("selat", [128, 2, 65], F32)
    P.dma("sp", selat[:], selat_d[:])
    qp = [P.sb("qp%d" % h, [65, L], BF16) for h in range(4)]
    kp = [P.sb("kp%d" % h, [65, L], BF16) for h in range(4)]
    for h in range(4):
        P.memset("pool", kp[h][64:65, :], 1.0)
    Vp = P.sb("Vp", [128, NT, 4, 128], BF16)
    P.memset("pool", Vp[:], 1.0)
    inb = Rot([P.sb("in", [128, L], F32) for _ in range(2)])
    sqb = Rot([P.sb("sqb", [128, L], F32) for _ in range(2)])
    vin = P.sb("vin", [128, NT, 256], F32)
    kmx = P.sb("kmx", [65, 4, 4], F32)
    kmax2 = P.sb("kmax2", [65, 4], F32)
    tmpr = Rot([P.sb("tmpr", [65, 512], F32) for _ in range(2)])
    Pt = Rot([P.sb("Pt", [128, 512], BF16) for _ in range(4)])
    PEt = Rot([P.sb("PEt", [128, 512], BF16) for _ in range(4)])
    rden = Rot([P.sb("rden", [128, 512], F32) for _ in range(2)])
    osb = P.sb("osb", [128, 2, L], BF16)
    psS = Rot([bank(P, "psS") for _ in range(4)])
    psO = Rot([bank(P, "psO") for _ in range(2)])
    psN = Rot([bank(P, "psN") for _ in range(2)])
    for s in range(nseq):
        c0 = s * L
        for (isq, cc) in ((0, 2), (0, 3), (1, 0), (1, 1)):
            x = inb.next()
            P.dma("sp", x[:], pT[R_AT + cc * 128:R_AT + (cc + 1) * 128, c0:c0 + L])
            c = cc % 2
            dst = qp if isq else kp
            sc = 0.125 if isq else 1.0
            P.act(dst[2 * c][0:64, :], x[0:64, :], AF.Copy, scale=sc)
            P.ts("dve", dst[2 * c + 1][0:64, :], x[64:128, :], sc, ALU.mult)
            sq = sqb.next()
            P.act(sq[:], x[:], AF.Square, scale=sc)
            for hh in range(2):
                h = 2 * c + hh
                for g in range(4):
                    cs = slice(g * 512, (g + 1) * 512)
                    pn = psN.next()
                    P.mm(pn[0:65, :], selat[:, hh, :], sq[:, cs])
                    if not isq:
                        P.reduce(kmx[:, h, g:g + 1], pn[0:65, :], ALU.max)
                    else:
                        tr = tmpr.next()
                        P.act(tr[64:65, :], pn[64:65, :], AF.Sqrt, scale=kmax2[64:65, h:h + 1])
                        P.ts("dve", qp[h][64:65, cs], tr[64:65, :], -1.0, ALU.mult)
                if not isq:
                    P.reduce(kmax2[:, h:h + 1], kmx[:, h, :], ALU.max)
                    P.ts("dve", kmax2[:, h:h + 1], kmax2[:, h:h + 1], 1.1, ALU.mult)
        P.dma("sp", vin[:], ptm[c0:c0 + L, C_AV:C_AV + 256].rr("(n p) c -> p n c", p=128))
        P.copy("pool", Vp[:, :, :, 0:64], vin[:].rr("p n (h e) -> p n h e", e=64))
        blocks = []
        for h in range(4):
            for g in range(4):
                js = [j for j in range(NT) if 4 * g - 8 <= j <= 4 * g + 11]
                for n_, j in enumerate(js):
                    blocks.append((h, g, j, n_ == 0, n_ == len(js) - 1))
        pend = []

        def issue_qk(b):
            h, g, j, _, _ = b
            p = psS.next()
            P.mm(p[:], kp[h][0:65, j * 128:(j + 1) * 128], qp[h][0:65, g * 512:(g + 1) * 512])
            pend.append(p)

        LOOK = 3
        for b in blocks[:LOOK]:
            issue_qk(b)
        po = None
        for bi, (h, g, j, first, last) in enumerate(blocks):
            if bi + LOOK < len(blocks):
                issue_qk(blocks[bi + LOOK])
            p = pend.pop(0)
            if first:
                po = psO.next()
            pt = Pt.next()
            P.act(pt[:], p[:], AF.Exp)
            pe = PEt.next()
            x0 = 128 * (4 * g - j + 15)
            P.tt("dve", pe[:], pt[:], strips[:, h, x0:x0 + 512], ALU.mult)
            P.mm(po[:], Vp[:, j, h, :], pe[:], start=first, stop=last)
            if last:
                rd = rden.next()
                P.recip(rd[64:128, :], po[64:128, :])
                hb = (h % 2) * 64
                P.tt("dve", osb[hb:hb + 64, h // 2, g * 512:(g + 1) * 512], po[0:64, :], rd[64:128, :], ALU.mult)
        P.dma("act", mixT[512:768, c0:c0 + L].rr("(c p) t -> p c t", p=128), osb[:])
    P.end()


TWO_PI = 2.0 * math.pi


def sin_layer(P, out, ps, freq, fb, tmp, m):
    P.ts("dve", tmp[:], ps, freq[:], ALU.mult, fb[:], ALU.add)
    P.ts("dve", m[:], tmp[:], math.pi, ALU.is_gt)
    P.stt(tmp[:], m[:], -TWO_PI, tmp[:], ALU.mult, ALU.add)
    P.ts("dve", m[:], tmp[:], -math.pi, ALU.is_lt)
    P.stt(tmp[:], m[:], TWO_PI, tmp[:], ALU.mult, ALU.add)
    P.act(out, tmp[:], AF.Sin)


def phase_HYF(P, zT_d, w1_d, w2_d, w3_d, pb_d, decf_d, decb_d, CTs_d, STs_d, Kd):
    P.begin("HYF")
    zT = P.sb("zT", [33, L], F32)
    w1 = P.sb("w1", [33, 64], F32)
    w2 = P.sb("w2", [64, 64], F32)
    w3 = P.sb("w3", [64, 512], F32)
    pb = P.sb("pb", [64, 3], F32)
    for (t_, d_) in ((zT, zT_d), (w1, w1_d), (w2, w2_d), (w3, w3_d), (pb, pb_d)):
        P.dma("sp", t_[:], d_[:])
    fb1 = P.sb("fb1", [64, 1], F32)
    fb2 = P.sb("fb2", [64, 1], F32)
    P.tt("dve", fb1[:], pb[:, 0:1], pb[:, 2:3], ALU.mult)
    P.tt("dve", fb2[:], pb[:, 1:2], pb[:, 2:3], ALU.mult)
    freq = pb[:, 2:3]
    h1 = P.sb("h1", [64, L], F32)
    h2 = P.sb("h2", [64, L], F32)
    tmp = Rot([P.sb("tmp", [64, 512], F32) for _ in range(2)])
    mm_ = Rot([P.sb("m", [64, 512], F32) for _ in range(2)])
    ps = Rot([bank(P, "ps") for _ in range(2)])
    for g in range(4):
        cs = slice(g * 512, (g + 1) * 512)
        p = ps.next()
        P.mm(p[0:64, :], w1[:], zT[:, cs])
        sin_layer(P, h1[:, cs], p[0:64, :], freq, fb1, tmp.next(), mm_.next())
    for g in range(4):
        cs = slice(g * 512, (g + 1) * 512)
        p = ps.next()
        P.mm(p[0:64, :], w2[:], h1[:, cs])
        sin_layer(P, h2[:, cs], p[0:64, :], freq, fb2, tmp.next(), mm_.next())
    hs = P.sb("hs", [128, NT, 256], BF16)
    hd = P.sb("hd", [128, NT, 256], BF16)
    dfb = Rot([P.sb("dfb", [128, 2, 256], F32) for _ in range(2)])
    hfb = Rot([P.sb("hfb", [128, 2, 256], F32) for _ in range(2)])
    for i in range(NT):
        df = dfb.next()
        P.dma("sp", df[:, 0, :], decf_d[i * 128:(i + 1) * 128, :])
        P.dma("sp", df[:, 1, :], decb_d[i * 128:(i + 1) * 128, :])
        p = ps.next()
        P.mm(p[:], h2[:, i * 128:(i + 1) * 128], w3[:])
        hf = hfb.next()
        P.tt("dve", hf[:], p[:].rr("p (a c) -> p a c", a=2), df[:], ALU.mult)
        P.tt("dve", hs[:, i, :], hf[:, 0, :], hf[:, 1, :], ALU.add)
        P.tt("dve", hd[:, i, :], hf[:, 1, :], hf[:, 0, :], ALU.subtract)
    ctb = Rot([P.sb("ctb", [128, NT, 128], BF16) for _ in range(2)])
    stb = Rot([P.sb("stb", [128, NT, 128], BF16) for _ in range(2)])
    kb = Rot([P.sb("kb", [128, 512], F32) for _ in range(2)])
    for j in range(16):
        ct, st = ctb.next(), stb.next()
        P.dma("sp", ct[:], CTs_d[j])
        P.dma("sp", st[:], STs_d[j])
        p = ps.next()
        for i in range(NT):
            P.mm(p[:, 0:256], ct[:, i, :], hs[:, i, :], start=(i == 0), stop=(i == NT - 1))
        for i in range(NT):
            P.mm(p[:, 256:512], st[:, i, :], hd[:, i, :], start=(i == 0), stop=(i == NT - 1))
        k_ = kb.next()
        P.copy("act", k_[:], p[:])
        P.dma("act", Kd[j], k_[:])
    P.end()


def phase_HY(P, nseq, pT, cw_d, fbias_d, Kd, CTs_d, STs_d, Ci_d, Si_d, mixT):
    P.begin("HY")
    ident = make_ident(P)
    cw = P.sb("cw", [128, 6, 3], F32)
    fbias = P.sb("fbias", [128, 2], F32)
    P.dma("sp", cw[:], cw_d[:])
    P.dma("sp", fbias[:], fbias_d[:])
    K = P.sb("K", [128, 16, 512], F32)
    P.dma("sp", K[:], Kd[:].rr("j p c -> p j c"))
    Pp = Rot([P.sb("Pp", [128, L + 2], F32) for _ in range(2)])
    for t_ in Pp.t:
        P.memset("pool", t_[:, 0:1], 0.0)
        P.memset("pool", t_[:, L + 1:L + 2], 0.0)
    x0 = P.sb("x0", [128, 2, L], F32)
    x1 = Rot([P.sb("x1", [128, L], F32) for _ in range(2)])
    vv = Rot([P.sb("vv", [128, L], F32) for _ in range(2)])
    wf = P.sb("wf", [128, 2, L], F32)
    wb = Rot([P.sb("wb", [128, L], BF16) for _ in range(2)])
    wT = P.sb("wT", [128, NT, 256], BF16)
    Y = P.sb("Y", [128, 16, 512], BF16)
    ctb = Rot([P.sb("ctb", [128, NT, 128], BF16) for _ in range(2)])
    stb = Rot([P.sb("stb", [128, NT, 128], BF16) for _ in range(2)])
    cib = Rot([P.sb("cib", [128, 1024], BF16) for _ in range(2)])
    sib = Rot([P.sb("sib", [128, 1024], BF16) for _ in range(2)])
    usb = Rot([P.sb("usb", [128, 512], F32) for _ in range(2)])
    t1 = Rot([P.sb("t1", [128, 256], F32) for _ in range(2)])
    t2 = Rot([P.sb("t2", [128, 256], F32) for _ in range(2)])
    ot = Rot([P.sb("ot", [128, 512], F32) for _ in range(2)])
    outb = P.sb("outb", [128, 2, L], BF16)
    psT = P.ps("psT", [128, 8, 128], BF16)
    psU = Rot([bank(P, "psU") for _ in range(2)])
    psI = [bank(P, "psI%d" % i) for i in range(4)]

    def conv3(dst, c, s):
        pp = Pp.next()
        P.dma("sp", pp[:, 1:L + 1], pT[R_HY + c * 128:R_HY + (c + 1) * 128, s * L:(s + 1) * L])
        P.ts("dve", dst, pp[:, 0:L], cw[:, c, 0:1], ALU.mult)
        P.stt(dst, pp[:, 1:L + 1], cw[:, c, 1:2], dst, ALU.mult, ALU.add)
        P.stt(dst, pp[:, 2:L + 2], cw[:, c, 2:3], dst, ALU.mult, ALU.add)

    for s in range(nseq):
        for c in range(2):
            conv3(x0[:, c, :], c, s)
            a, b = x1.next(), vv.next()
            conv3(a[:], 2 + c, s)
            conv3(b[:], 4 + c, s)
            P.tt("pool", wf[:, c, :], a[:], b[:], ALU.mult)
            w_ = wb.next()
            P.copy("act", w_[:], wf[:, c, :])
            for T8 in range(0, NT, 8):
                for j in range(8):
                    P.transpose(psT[:, j, :], w_[:, (T8 + j) * 128:(T8 + j + 1) * 128], ident[:])
                P.copy("act", wT[:, T8:T8 + 8, c * 128:(c + 1) * 128], psT[:])
        for j in range(16):
            ct, st = ctb.next(), stb.next()
            P.dma("sp", ct[:], CTs_d[j])
            P.dma("sp", st[:], STs_d[j])
            p = psU.next()
            for i in range(NT):
                P.mm(p[:, 0:256], ct[:, i, :], wT[:, i, :], start=(i == 0), stop=(i == NT - 1))
            for i in range(NT):
                P.mm(p[:, 256:512], st[:, i, :], wT[:, i, :], start=(i == 0), stop=(i == NT - 1))
            u = usb.next()
            P.copy("act", u[:], p[:])
            ur, us = u[:, 0:256], u[:, 256:512]
            kr, ki = K[:, j, 0:256], K[:, j, 256:512]
            a, b = t1.next(), t2.next()
            P.tt("dve", a[:], ur, kr, ALU.mult)
            P.tt("pool", b[:], us, ki, ALU.mult)
            P.tt("dve", Y[:, j, 0:256], a[:], b[:], ALU.add)
            a, b = t1.next(), t2.next()
            P.tt("dve", a[:], us, kr, ALU.mult)
            P.tt("pool", b[:], ur, ki, ALU.mult)
            P.tt("dve", Y[:, j, 256:512], a[:], b[:], ALU.subtract)
        for nh in range(2):
            for j in range(16):
                ci, si = cib.next(), sib.next()
                P.dma("sp", ci[:], Ci_d[j, :, nh * 1024:(nh + 1) * 1024])
                P.dma("sp", si[:], Si_d[j, :, nh * 1024:(nh + 1) * 1024])
                for cc in range(2):
                    for ng in range(2):
                        p = psI[cc * 2 + ng]
                        P.mm(p[:], Y[:, j, cc * 128:(cc + 1) * 128], ci[:, ng * 512:(ng + 1) * 512],
                             start=(j == 0), stop=False)
                        P.mm(p[:], Y[:, j, 256 + cc * 128:256 + (cc + 1) * 128], si[:, ng * 512:(ng + 1) * 512],
                             start=False, stop=(j == 15))
            for cc in range(2):
                for ng in range(2):
                    ns = slice(nh * 1024 + ng * 512, nh * 1024 + (ng + 1) * 512)
                    o = ot.next()
                    P.stt(o[:], wf[:, cc, ns], fbias[:, cc:cc + 1], psI[cc * 2 + ng][:], ALU.mult, ALU.add)
                    P.tt("dve", outb[:, cc, ns], o[:], x0[:, cc, ns], ALU.mult)
        P.dma("act", mixT[0:256, s * L:(s + 1) * L].rr("(c p) t -> p c t", p=128), outb[:])
    P.end()


def phase_O(P, nseq, mixT, w_out, Xin, Xout, hn2, LG, wr_d, nw2_d):
    P.begin("O")
    identf = make_ident(P, F32)
    Wo = P.sb("Wo", [128, 8, D], BF16)
    stg = Rot([P.sb("stg", [128, D], F32) for _ in range(2)])
    for k in range(8):
        st = stg.next()
        P.dma("sp", st[:], w_out[k * 128:(k + 1) * 128, :])
        P.copy(("pool", "dve", "act")[k % 3], Wo[:, k, :], st[:])
    nw2 = P.sb("nw2", [128, 8], F32)
    Wr = P.sb("Wr", [128, 8, 16], F32)
    P.dma("sp", nw2[:], nw2_d[:])
    P.dma("sp", Wr[:], wr_d[:])
    P.tt("dve", Wr[:], Wr[:], nw2[:].unsq(2).bcast([128, 8, 16]), ALU.mult)
    mTb = Rot([P.sb("mT", [128, 8, 512], BF16) for _ in range(2)])
    xb = Rot([P.sb("x", [128, D], F32) for _ in range(2)])
    x1b = Rot([P.sb("x1", [128, D], F32) for _ in range(2)])
    sq = P.sb("sq", [128, D], F32)
    ssb = Rot([P.sb("ss", [128, 1], F32) for _ in range(2)])
    rsb = Rot([P.sb("rs", [128, 1], F32) for _ in range(2)])
    hnf = Rot([P.sb("hnf", [128, D], F32) for _ in range(3)])
    hnb = Rot([P.sb("hnb", [128, D], BF16) for _ in range(2)])
    hT = Rot([P.sb("hT", [128, 8, 128], F32) for _ in range(2)])
    lgrow = Rot([P.sb("lgrow", [16, L], F32) for _ in range(2)])
    psO = Rot([bank(P, "psO") for _ in range(4)])
    psT = Rot([bank(P, "psT") for _ in range(2)])
    psR = bank(P, "psR")
    mview = mixT[:].rr("(k p) t -> p k t", p=128)
    tiles = [(s_, g, i4) for s_ in range(nseq) for g in range(L // 512) for i4 in range(4)]
    mts = {}
    lgs = {}

    def stageA(s_, g, i4):
        tg = s_ * L + g * 512
        if i4 == 0:
            mts[(s_, g)] = mTb.next()
            P.dma("sp", mts[(s_, g)][:], mview[:, :, tg:tg + 512])
        mT = mts[(s_, g)]
        t0 = tg + i4 * 128
        x, x1 = xb.next(), x1b.next()
        P.dma("sp", x[:], Xin[t0:t0 + 128, :])
        for half in range(2):
            p = psO.next()
            for k in range(8):
                P.mm(p[:], mT[:, k, i4 * 128:(i4 + 1) * 128], Wo[:, k, half * 512:(half + 1) * 512],
                     start=(k == 0), stop=(k == 7))
            P.tt("dve", x1[:, half * 512:(half + 1) * 512], x[:, half * 512:(half + 1) * 512], p[:], ALU.add)
        P.dma("pool", Xout[t0:t0 + 128, :], x1[:])
        ss, rs = ssb.next(), rsb.next()
        P.act(sq[:], x1[:], AF.Square, accum=ss[:])
        rms_rstd(P, ss, rs, D)
        hf, hb = hnf.next(), hnb.next()
        P.ts("dve", hf[:], x1[:], rs[:], ALU.mult)
        P.copy("pool", hb[:], hf[:])
        P.dma("pool", hn2[t0:t0 + 128, :], hb[:])
        return hf

    def stageB(s_, g, i4, hf):
        if g == 0 and i4 == 0:
            lgs[s_] = lgrow.next()
        lg = lgs[s_]
        h_ = hT.next()
        for k4 in range(2):
            pt = psT.next()
            for k in range(4):
                P.transpose(pt[:, k * 128:(k + 1) * 128], hf[:, (k4 * 4 + k) * 128:(k4 * 4 + k + 1) * 128], identf[:])
            P.copy("act", h_[:, k4 * 4:(k4 + 1) * 4, :], pt[:].rr("p (k t) -> p k t", k=4))
        for k in range(8):
            P.mm(psR[0:16, 0:128], Wr[:, k, :], h_[:, k, :], start=(k == 0), stop=(k == 7))
        tl = g * 512 + i4 * 128
        P.copy("act", lg[:, tl:tl + 128], psR[0:16, 0:128])
        if g == L // 512 - 1 and i4 == 3:
            P.dma("pool", LG[s_ * 16:(s_ + 1) * 16, :], lg[:])

    hf_next = stageA(*tiles[0])
    for ti, tl_ in enumerate(tiles):
        hf_cur = hf_next
        if ti + 1 < len(tiles):
            hf_next = stageA(*tiles[ti + 1])
        stageB(*tl_, hf_cur)
    P.end()


def phase_R(P, nseq, LG, blk_d, offs_d, GT, IT):
    P.begin("R")
    R_ = nseq * 16
    identf = make_ident(P, F32)
    lg = P.sb("lg", [R_, L], F32)
    P.dma("sp", lg[:], LG[:])
    blk = P.sb("blk", [64, 64], F32)
    offs = P.sb("offs", [64, 1], F32)
    P.dma("sp", blk[:], blk_d[:])
    P.dma("sp", offs[:], offs_d[:])
    P.ts("dve", lg[:], lg[:], 80.0, ALU.min)
    e = P.sb("e", [R_, L], F32)
    P.act(e[:], lg[:], AF.Exp)
    aff = P.sb("aff", [R_, L], F32)
    rd = P.sb("rd", [R_, 512], F32)
    ps = Rot([bank(P, "ps") for _ in range(2)])
    for g in range(4):
        cs = slice(g * 512, (g + 1) * 512)
        p = ps.next()
        P.mm(p[0:R_, :], blk[0:R_, 0:R_], e[:, cs])
        P.recip(rd[:], p[0:R_, :])
        P.tt("dve", aff[:, cs], e[:, cs], rd[:], ALU.mult)
    gates = P.sb("gates", [R_, 256], F32)
    idx = P.sb("idx", [R_, 256], U32)
    for it in range(32):
        m8 = gates[:, it * 8:(it + 1) * 8]
        i8 = idx[:, it * 8:(it + 1) * 8]
        P.generic("dve", (lambda m8=m8: lambda en: en.max(m8.ap, aff.h[:, :]))(), [aff[:]], [m8])
        P.generic("dve", (lambda m8=m8, i8=i8: lambda en: en.max_index(i8.ap, m8.ap, aff.h[:, :]))(), [aff[:], m8], [i8])
        P.generic("dve", (lambda m8=m8: lambda en: en.match_replace(aff.h[:, :], m8.ap, aff.h[:, :], -1.0))(),
                  [aff[:], m8], [aff[:]])
    idf = P.sb("idf", [R_, 256], F32)
    P.copy("dve", idf[:], idx[:])
    P.ts("dve", idf[:], idf[:], offs[0:R_, :], ALU.add)
    gts = P.sb("gts", [128, 2, 64], F32)
    its = P.sb("its", [128, 2, 64], I32)
    P.memset("pool", gts[:], 0.0)
    P.memset("pool", its[:], 0)
    for j2 in range(2):
        p = ps.next()
        P.transpose(p[:, 0:R_], gates[:, j2 * 128:(j2 + 1) * 128], identf[0:R_, 0:R_])
        P.copy("act", gts[:, j2, 0:R_], p[:, 0:R_])
        p = ps.next()
        P.transpose(p[:, 0:R_], idf[:, j2 * 128:(j2 + 1) * 128], identf[0:R_, 0:R_])
        P.copy("dve", its[:, j2, 0:R_], p[:, 0:R_])
    P.dma("sp", GT[:], gts[:])
    P.dma("sp", IT[:], its[:])
    P.end()


def phase_E(P, nseq, wg_d, wu_d, wd_d, nw2_d, hn2, GT, IT, Xout, experts=range(16)):
    P.begin("E")
    identb = make_ident(P)
    gT = P.sb("gT", [128, 2, 64], F32)
    iT = P.sb("iT", [128, 2, 64], I32)
    nw2 = P.sb("nw2", [128, 8], F32)
    P.dma("sp", gT[:], GT[:])
    P.dma("sp", iT[:], IT[:])
    P.dma("sp", nw2[:], nw2_d[:])
    NSL = nseq * 256
    Wg = Rot([P.sb("Wg", [128, 8, D], BF16) for _ in range(2)])
    Wu = Rot([P.sb("Wu", [128, 8, D], BF16) for _ in range(2)])
    Wd = Rot([P.sb("Wd", [128, 8, D], BF16) for _ in range(2)])
    stg = Rot([P.sb("stg", [128, D], F32) for _ in range(6)])
    xeb = Rot([P.sb("xe", [128, D], BF16) for _ in range(3)])
    xeTb = Rot([P.sb("xeT", [128, 8, NSL], BF16) for _ in range(2)])
    hT = P.sb("hT", [128, 8, NSL], BF16)
    sil = Rot([P.sb("sil", [128, 512], F32) for _ in range(2)])
    yeb = Rot([P.sb("ye", [128, D], F32) for _ in range(2)])
    psT = Rot([P.ps("psT", [128, 8, 128], BF16) for _ in range(2)])
    psG = Rot([bank(P, "psG") for _ in range(2)])
    psU = Rot([bank(P, "psU") for _ in range(2)])
    psY = Rot([bank(P, "psY") for _ in range(2)])
    IOA = bass.IndirectOffsetOnAxis
    experts = list(experts)

    def prep_w(e):
        wg, wu, wd = Wg.next(), Wu.next(), Wd.next()
        groups = []
        for k in range(8):
            def grp(k=k):
                st = stg.next()
                P.dma("sp", st[:], wg_d[e, k * 128:(k + 1) * 128, :])
                P.ts("dve", wg[:, k, :], st[:], nw2[:, k:k + 1], ALU.mult)
                st = stg.next()
                P.dma("sp", st[:], wu_d[e, k * 128:(k + 1) * 128, :])
                P.act(wu[:, k, :], st[:], AF.Copy, scale=nw2[:, k:k + 1])
                st = stg.next()
                P.dma("sp", st[:], wd_d[e, k * 128:(k + 1) * 128, :])
                P.copy("dve" if k % 2 == 0 else "act", wd[:, k, :], st[:])
            groups.append(grp)
        return (wg, wu, wd), groups

    def prep_x(e):
        xeT = xeTb.next()
        for s in range(nseq):
            col = s * 16 + e
            for j2 in range(2):
                xe = xeb.next()
                ix = iT[:, j2, col:col + 1]
                P.dma_generic("pool", (lambda xe=xe, ix=ix: lambda en: en.indirect_dma_start(
                    out=xe.h[:, :], out_offset=None, in_=hn2.h.ap()[:, :], in_offset=IOA(ap=ix.ap, axis=0)))(),
                    [ix, hn2[:]], [xe[:]])
                pt = psT.next()
                for k in range(8):
                    P.transpose(pt[:, k, :], xe[:, k * 128:(k + 1) * 128], identb[:])
                sl0 = (s * 2 + j2) * 128
                P.copy("act" if j2 == 0 else "dve", xeT[:, :, sl0:sl0 + 128], pt[:])
        return xeT

    wcur, groups = prep_w(experts[0])
    for g_ in groups:
        g_()
    xcur = prep_x(experts[0])
    for ei, e in enumerate(experts):
        wg, wu, wd = wcur
        xeT = xcur
        groups = []
        if ei + 1 < len(experts):
            wcur, groups = prep_w(experts[ei + 1])
        for fc in range(8):
            for sg in range(0, NSL, 512):
                n = min(512, NSL - sg)
                pg, pu = psG.next(), psU.next()
                for k in range(8):
                    P.mm(pg[:, 0:n], wg[:, k, fc * 128:(fc + 1) * 128], xeT[:, k, sg:sg + n], start=(k == 0), stop=(k == 7))
                for k in range(8):
                    P.mm(pu[:, 0:n], wu[:, k, fc * 128:(fc + 1) * 128], xeT[:, k, sg:sg + n], start=(k == 0), stop=(k == 7))
                sl = sil.next()
                P.act(sl[:, 0:n], pg[:, 0:n], AF.Silu)
                P.tt("dve", hT[:, fc, sg:sg + n], sl[:, 0:n], pu[:, 0:n], ALU.mult)
            if groups:
                groups.pop(0)()
        if ei + 1 < len(experts):
            xcur = prep_x(experts[ei + 1])
        for s in range(nseq):
            col = s * 16 + e
            for j2 in range(2):
                sl0 = (s * 2 + j2) * 128
                ye = yeb.next()
                for half in range(2):
                    py = psY.next()
                    for fc in range(8):
                        P.mm(py[:], hT[:, fc, sl0:sl0 + 128], wd[:, fc, half * 512:(half + 1) * 512],
                             start=(fc == 0), stop=(fc == 7))
                    if half == 0:
                        P.act(ye[:, 0:512], py[:], AF.Copy, scale=gT[:, j2, col:col + 1])
                    else:
                        P.ts("dve", ye[:, 512:1024], py[:], gT[:, j2, col:col + 1], ALU.mult)
                ix = iT[:, j2, col:col + 1]
                P.dma_generic("pool", (lambda ye=ye, ix=ix: lambda en: en.indirect_dma_start(
                    out=Xout.h.ap()[:, :], out_offset=IOA(ap=ix.ap, axis=0), in_=ye.h[:, :], in_offset=None,
                    compute_op=ALU.add, oob_is_err=True))(),
                    [ix, ye[:]], [Xout[:]])
    P.end()


def phase_F(P, T, Xin, fw_d, out):
    P.begin("F")
    fw = P.sb("fw", [128, D], F32)
    P.dma("sp", fw[:], fw_d[:])
    xb = Rot([P.sb("x", [128, D], F32) for _ in range(3)])
    ob = Rot([P.sb("o", [128, D], F32) for _ in range(3)])
    sq = P.sb("sq", [128, D], F32)
    ssb = Rot([P.sb("ss", [128, 1], F32) for _ in range(2)])
    rsb = Rot([P.sb("rs", [128, 1], F32) for _ in range(2)])
    for i in range(T // 128):
        x, o = xb.next(), ob.next()
        P.dma("sp", x[:], Xin[i * 128:(i + 1) * 128, :])
        ss, rs = ssb.next(), rsb.next()
        P.act(sq[:], x[:], AF.Square, accum=ss[:])
        rms_rstd(P, ss, rs, D)
        P.stt(o[:], x[:], rs[:], fw[:], ALU.mult, ALU.mult)
        P.dma("pool", out[i * 128:(i + 1) * 128, :], o[:])
    P.end()


BF = ml_dtypes.bfloat16
_CONST_CACHE = {}


def host_consts():
    if _CONST_CACHE:
        return _CONST_CACHE
    c = {}
    c["masks"] = gla_masks()
    selh = np.zeros((8, 2, 2, 128), np.float32)
    for d in range(2):
        for cc in range(2):
            for m in range(128):
                selh[d * 4 + 2 * cc + m // 64, d, cc, m] = 1.0
    c["selh"] = selh
    selg = np.zeros((128, 2, 128), np.float32)
    for cc in range(2):
        for m in range(128):
            selg[cc * 64 + m % 64, cc, m] = 1.0
    c["selg"] = selg.astype(BF)
    selat = np.zeros((128, 2, 65), np.float32)
    selat[0:64, 0, :] = 1.0
    selat[64:128, 1, :] = 1.0
    c["selat"] = selat
    z = np.arange(4096)
    rel = (2047 - z).astype(np.int64)
    nb, max_exact = 16, 8
    ret = (rel > 0).astype(np.int32) * nb
    n = np.abs(rel)
    nf = np.maximum(n, 1).astype(np.float32)
    large = max_exact + (np.log(nf / np.float32(max_exact)) / np.float32(math.log(1024 / max_exact))
                         * np.float32(nb - max_exact)).astype(np.int32)
    large = np.minimum(large, nb - 1)
    bucket = ret + np.where(n < max_exact, n, large)
    oh = np.zeros((32, 4096), np.float32)
    oh[bucket, z] = 1.0
    c["onehot"] = oh
    mult = ((n <= 64).astype(np.float32) + ((n % 4 == 0) & (n <= 256)).astype(np.float32)
            + ((n % 16 == 0) & (n <= 1024)).astype(np.float32))
    c["multrep"] = np.ascontiguousarray(np.broadcast_to(mult[None, :], (128, 4096))).astype(np.float32)
    t = np.linspace(0.0, 1.0, L, dtype=np.float32)[:, None]
    ang_pos = (np.float32(2.0 * math.pi) * np.arange(L, dtype=np.float32) / np.float32(L)).astype(np.float32)
    f = np.linspace(1e-4, 15, 16, dtype=np.float32)
    ang = (ang_pos[:, None] * f[None, :]).astype(np.float32)
    zf = np.concatenate([t, np.cos(ang), -np.sin(ang)], axis=-1).astype(np.float32)
    c["zT"] = np.ascontiguousarray(zf.T)
    max_decay = math.log(1e-2) / 0.3
    min_decay = math.log(1e-2) / 1.5
    deltas = np.abs(np.linspace(min_decay, max_decay, 256, dtype=np.float32))
    dec = np.exp(-t * deltas[None, :]).astype(np.float32)
    c["decf"] = dec
    decb = dec.copy()
    decb[0] = 0.0
    c["decb"] = decb
    ff = np.arange(2048, dtype=np.float64)
    th = np.pi * (2 * ff[:, None] + 1) * ff[None, :] / 4096.0
    C, S = np.cos(th), np.sin(th)
    c["CTs"] = np.ascontiguousarray(C.reshape(16, 128, 16, 128).transpose(0, 3, 2, 1)).astype(BF)
    c["STs"] = np.ascontiguousarray(S.reshape(16, 128, 16, 128).transpose(0, 3, 2, 1)).astype(BF)
    c["Ci"] = (C * (2.0 / 4096.0)).reshape(16, 128, 2048).astype(BF)
    c["Si"] = (S * (2.0 / 4096.0)).reshape(16, 128, 2048).astype(BF)
    blk = np.zeros((64, 64), np.float32)
    for a in range(4):
        blk[a * 16:(a + 1) * 16, a * 16:(a + 1) * 16] = 1.0
    c["blk"] = blk
    c["offs"] = ((np.arange(64) // 16) * 2048).astype(np.float32)[:, None]
    _CONST_CACHE.update(c)
    return c


CONST_SPECS = [("masks", [2, 128, 256], U32), ("selh", [8, 2, 2, 128], F32), ("selg", [128, 2, 128], BF16),
               ("selat", [128, 2, 65], F32), ("onehot", [32, 4096], F32), ("multrep", [128, 4096], F32),
               ("zT", [33, L], F32), ("decf", [L, 256], F32), ("decb", [L, 256], F32),
               ("CTs", [16, 128, 16, 128], BF16), ("STs", [16, 128, 16, 128], BF16),
               ("Ci", [16, 128, 2048], BF16), ("Si", [16, 128, 2048], BF16),
               ("blk", [64, 64], F32), ("offs", [64, 1], F32)]


def pk(v):
    return np.ascontiguousarray(np.asarray(v).reshape(-1, 128).T)


def layer_params(inp, l):
    d = {}
    d["nw1"] = pk(inp["norm_mix_w"][l])
    d["nw2"] = pk(inp["norm_ffn_w"][l])
    d["hy_cw"] = np.ascontiguousarray(inp["hy_conv_w"][l].reshape(3, 6, 128).transpose(2, 1, 0))
    d["hy_w1"] = np.ascontiguousarray(inp["hy_pos_w1"][l])
    d["hy_w2"] = np.ascontiguousarray(inp["hy_pos_w2"][l])
    d["hy_w3"] = np.ascontiguousarray(inp["hy_pos_w3"][l])
    d["hy_pb"] = np.ascontiguousarray(np.stack([inp["hy_pos_b1"][l], inp["hy_pos_b2"][l], inp["hy_sin_freq"][l]], 1))
    d["hy_fb"] = np.ascontiguousarray(inp["hy_filt_bias"][l].reshape(2, 128).T)
    d["m_cw"] = np.ascontiguousarray(inp["m_conv_w"][l].reshape(5, 4, 128).transpose(2, 1, 0))
    d["m_cb"] = np.ascontiguousarray(inp["m_conv_b"][l].reshape(4, 128).T)
    d["m_dtb"] = np.ascontiguousarray(inp["m_dt_bias"][l].reshape(8, 1))
    d["m_alog"] = np.ascontiguousarray(inp["m_A_log"][l].reshape(8, 1))
    d["m_dsk"] = np.ascontiguousarray(np.broadcast_to(np.repeat(inp["m_D"][l], 64)[None, :], (128, 256)))
    d["m_nw"] = np.ascontiguousarray(np.broadcast_to(inp["m_norm_w"][l][None, :], (128, 256)))
    d["hg_nw"] = np.ascontiguousarray(np.broadcast_to(np.tile(inp["hg_norm_w"][l], 4)[None, :], (128, 256)))
    d["wr"] = np.ascontiguousarray(inp["router_w"][l].reshape(8, 128, 16).transpose(1, 0, 2))
    return {k: np.asarray(v, np.float32) for k, v in d.items()}


LAYER_SPECS = [("nw1", [128, 8]), ("nw2", [128, 8]), ("hy_cw", [128, 6, 3]), ("hy_w1", [33, 64]), ("hy_w2", [64, 64]),
               ("hy_w3", [64, 512]), ("hy_pb", [64, 3]), ("hy_fb", [128, 2]), ("m_cw", [128, 4, 5]), ("m_cb", [128, 4]),
               ("m_dtb", [8, 1]), ("m_alog", [8, 1]), ("m_dsk", [128, 256]), ("m_nw", [128, 256]), ("hg_nw", [128, 256]),
               ("wr", [128, 8, 16])]


def global_params(inp):
    d = {}
    d["rbrep"] = np.ascontiguousarray(np.broadcast_to(inp["rel_bias"].T[:, :, None], (4, 32, 128))).astype(np.float32)
    d["lbraw"] = np.ascontiguousarray(inp["hg_lb"].reshape(2, 2, 2, 128).transpose(3, 0, 1, 2)).astype(np.float32)
    d["fw"] = np.ascontiguousarray(np.broadcast_to(inp["final_norm_w"][None, :], (128, 1024))).astype(np.float32)
    return d


GLOBAL_SPECS = [("rbrep", [4, 32, 128]), ("lbraw", [128, 2, 2, 2]), ("fw", [128, 1024])]
DEPTH = 2


def build_program(nseq, depth=DEPTH, do_moe=True, do_mix=True, debug=None, experts=range(16)):
    T = nseq * L
    P = Prog()
    I = {}

    def inp(name, shape, dt):
        I[name] = P.dram(name, shape, dt, kind="ExternalInput")
        return I[name]

    x = inp("x", [T, D], F32)
    w_in = inp("w_in", [depth, D, 3592], F32)
    w_out = inp("w_out", [depth, D, D], F32)
    if do_moe:
        wg = inp("moe_w_gate", [depth, 16, D, D], F32)
        wu = inp("moe_w_up", [depth, 16, D, D], F32)
        wd = inp("moe_w_down", [depth, 16, D, D], F32)
    for (n, shp, dt) in CONST_SPECS:
        inp("c_" + n, shp, dt)
    for (n, shp) in GLOBAL_SPECS:
        inp("g_" + n, shp, F32)
    for l in range(depth):
        for (n, shp) in LAYER_SPECS:
            inp("l%d_%s" % (l, n), shp, F32)
    out = P.dram("out", [T, D], F32, kind="ExternalOutput")
    dbg = {}
    if debug:
        for (n, shp, dt) in debug:
            dbg[n] = P.dram("dbg_" + n, shp, dt, kind="ExternalOutput")

    def scratch(name, shape, dt):
        if name in dbg:
            return dbg[name]
        return P.dram("s_" + name, shape, dt)

    X = [x] + [scratch("X%d" % (l + 1), [T, D], F32) for l in range(depth)]
    pT = scratch("pT", [NFM, T], F32)
    ptm = scratch("ptm", [T, NTM], F32)
    mixT = scratch("mixT", [D, T], BF16) if do_mix else inp("mixT_in", [D, T], BF16)
    gq = scratch("gq", [256, T], BF16)
    gk = scratch("gk", [2, 256, T], BF16)
    gg = scratch("gg", [2, 256, T], F32)
    gv = scratch("gv", [T, 256], BF16)
    go = scratch("go", [T, 256], F32)
    Kd = scratch("Kd", [16, 128, 512], F32)
    Mtab = scratch("Mtab", [4, 128, 4096], BF16)
    hn2 = scratch("hn2", [T, D], BF16)
    LG = scratch("LG", [nseq * 16, L], F32)
    GT = scratch("GT", [128, 2, 64], F32)
    IT = scratch("IT", [128, 2, 64], I32)
    C = lambda n: I["c_" + n]
    if do_mix:
        phase_ATE(P, I["g_rbrep"], C("onehot"), C("multrep"), Mtab)
    for l in range(depth):
        Lp = lambda n: I["l%d_%s" % (l, n)]
        if do_mix:
            phase_P(P, T, X[l], w_in[l], Lp("nw1"), pT, ptm)
            phase_HYF(P, C("zT"), Lp("hy_w1"), Lp("hy_w2"), Lp("hy_w3"), Lp("hy_pb"), C("decf"), C("decb"),
                      C("CTs"), C("STs"), Kd)
            phase_HY(P, nseq, pT, Lp("hy_cw"), Lp("hy_fb"), Kd, C("CTs"), C("STs"), C("Ci"), C("Si"), mixT)
            phase_AT(P, nseq, pT, ptm, Mtab, C("selat"), mixT)
            phase_MApre(P, nseq, pT, Lp("m_cw"), Lp("m_cb"), Lp("m_dtb"), Lp("m_alog"), C("selh"), C("selg"),
                        gq, gk, gg, gv)
            phase_GLA(P, nseq, L, gq, gk, gg, gv, go, C("masks"))
            phase_MApost(P, T, go, gv, ptm, Lp("m_dsk"), Lp("m_nw"), mixT)
            phase_HGpre(P, nseq, l, pT, ptm, I["g_lbraw"], gq, gk, gg, gv)
            phase_GLA(P, nseq, L, gq, gk, gg, gv, go, C("masks"))
            phase_HGpost(P, T, go, ptm, Lp("hg_nw"), mixT)
        phase_O(P, nseq, mixT, w_out[l], X[l], X[l + 1], hn2, LG, Lp("wr"), Lp("nw2"))
        if do_moe:
            phase_R(P, nseq, LG, C("blk"), C("offs"), GT, IT)
            phase_E(P, nseq, wg[l], wu[l], wd[l], Lp("nw2"), hn2, GT, IT, X[l + 1], experts=experts)
    phase_F(P, T, X[depth], I["g_fw"], out)
    nc = P.finish()
    return P, nc, list(I.keys())


def host_inputs(inp, depth=DEPTH):
    m = {}
    for k, v in host_consts().items():
        m["c_" + k] = v
    for k, v in global_params(inp).items():
        m["g_" + k] = v
    for l in range(depth):
        for k, v in layer_params(inp, l).items():
            m["l%d_%s" % (l, k)] = v
    return m


N_CORES = 8
SEQ_PER_CORE = 4
_PROG = {}


def kernel(**inputs):
    inp = {k: np.asarray(v) for k, v in inputs.items()}
    if "p" not in _PROG:
        _PROG["p"] = build_program(SEQ_PER_CORE)
    P, nc, names = _PROG["p"]
    rep = host_inputs(inp)
    for k in ("w_in", "w_out", "moe_w_gate", "moe_w_up", "moe_w_down"):
        rep[k] = np.ascontiguousarray(inp[k], dtype=np.float32)
    x = np.ascontiguousarray(inp["x"], dtype=np.float32)
    in_maps = []
    for c in range(N_CORES):
        m = dict(rep)
        m["x"] = x[c * SEQ_PER_CORE:(c + 1) * SEQ_PER_CORE].reshape(SEQ_PER_CORE * L, D)
        in_maps.append({k: m[k] for k in names})
    res = run_bass_kernel_spmd(nc, in_maps, core_ids=list(range(N_CORES)))
    out = np.concatenate([np.asarray(r["out"]).reshape(SEQ_PER_CORE, L, D) for r in res.results], axis=0)
    return np.ascontiguousarray(out.astype(np.float32))
```
